# Optimizing a Trainium2 kernel written in Bass

```python
import math
import jax, jax.numpy as jnp
from jax import lax
import numpy as np

D_MODEL = 1024
BATCH = 2
SEQ = 16384
DEPTH = 4

HEAD_DIM = 64
FOX_W = D_MODEL // 2
RWKV_W = D_MODEL // 4
RET_W = D_MODEL // 4
MIX_W = FOX_W + RWKV_W + RET_W
FOX_HEADS = FOX_W // HEAD_DIM
RWKV_HEADS = RWKV_W // HEAD_DIM
RET_HEADS = RET_W // HEAD_DIM
Q_BLOCK = 128
RET_CHUNK = 128
W_LORA = 64
A_LORA = 64
G_LORA = 128
LORA_W = W_LORA + A_LORA + G_LORA
ROPE_BASE = 10000.0
IN_SPLITS = (FOX_W, FOX_W, FOX_W, FOX_HEADS,
             RWKV_W, RWKV_W, RWKV_W, W_LORA, A_LORA, G_LORA,
             RET_W, RET_W, RET_W, RET_W)
IN_W = sum(IN_SPLITS)
N_GROUPS = 4
EXPERTS_PER_GROUP = 8
N_EXPERTS = N_GROUPS * EXPERTS_PER_GROUP
TOP_K = 2
D_EXPERT = 512
MOE_BLOCK = 128
ALPHA = (2.0 * DEPTH) ** 0.25
BETA = (8.0 * DEPTH) ** -0.25
LN_EPS = 1e-5
RWKV_GN_EPS = 64e-5
RET_GN_EPS = 1e-6

kernel_name = "hybrid_fox_rwkv7_retnet_hmoe_deepnorm"

F32 = jnp.float32


def split_columns(p, sizes):
    outs = []
    start = 0
    for s in sizes:
        outs.append(p[..., start:start + s])
        start += s
    return outs


def layer_norm(x, g, b):
    xf = x.astype(F32)
    mu = jnp.mean(xf, -1, keepdims=True)
    var = jnp.mean(jnp.square(xf - mu), -1, keepdims=True)
    return ((xf - mu) * lax.rsqrt(var + LN_EPS) * g + b).astype(x.dtype)


def head_norm(y, eps):
    yf = y.astype(F32)
    mu = jnp.mean(yf, -1, keepdims=True)
    var = jnp.mean(jnp.square(yf - mu), -1, keepdims=True)
    return (yf - mu) * lax.rsqrt(var + eps)


def fox_attention(q, k, v, f_logit):
    B_, S_, H, Dh = q.shape
    c = jnp.cumsum(jax.nn.log_sigmoid(f_logit.astype(F32)), axis=1)
    c = c.transpose(0, 2, 1)
    qh = q.transpose(0, 2, 1, 3)
    kh = k.transpose(0, 2, 1, 3)
    vh = v.transpose(0, 2, 1, 3)
    scale = Dh ** -0.5
    kpos = jnp.arange(S_)
    n_blk = S_ // Q_BLOCK

    def one_block(i):
        start = i * Q_BLOCK
        qb = lax.dynamic_slice_in_dim(qh, start, Q_BLOCK, axis=2)
        cb = lax.dynamic_slice_in_dim(c, start, Q_BLOCK, axis=2)
        s = jnp.einsum('bhqd,bhkd->bhqk', qb, kh).astype(F32) * scale
        s = s + cb[..., :, None] - c[..., None, :]
        qpos = start + jnp.arange(Q_BLOCK)
        s = jnp.where(kpos[None, :] <= qpos[:, None], s, -jnp.inf)
        p = jax.nn.softmax(s, axis=-1).astype(vh.dtype)
        return jnp.einsum('bhqk,bhkd->bhqd', p, vh)

    out = lax.map(one_block, jnp.arange(n_blk))
    out = out.transpose(1, 0, 3, 2, 4)
    return out.reshape(B_, S_, H * Dh)


def rwkv7_time_mix(r, k, v, wd, ad, gd, mu_rkv, mu_lora, w0, w2, a0, a2, g2,
                   k_k, k_a, r_k, ln_g, ln_b):
    B_, S_, _ = r.shape
    H, N = RWKV_HEADS, HEAD_DIM

    def shift_mix(p, mu):
        prev = jnp.pad(p, ((0, 0), (1, 0), (0, 0)))[:, :-1]
        return p + (prev - p) * mu

    r = shift_mix(r, mu_rkv[0]).astype(F32)
    k = shift_mix(k, mu_rkv[1]).astype(F32)
    v = shift_mix(v, mu_rkv[2]).astype(F32)
    lora = shift_mix(jnp.concatenate([wd, ad, gd], axis=-1), mu_lora).astype(F32)
    wd, ad, gd = split_columns(lora, (W_LORA, A_LORA, G_LORA))
    log_w = -jax.nn.softplus(-(w0 + jnp.tanh(wd) @ w2)) - 0.5
    decay = jnp.exp(-jnp.exp(log_w))
    a = jax.nn.sigmoid(a0 + ad @ a2)
    g = jax.nn.sigmoid(gd) @ g2
    kk = (k * k_k).reshape(B_, S_, H, N)
    kk = kk / jnp.maximum(jnp.sqrt(jnp.sum(kk * kk, -1, keepdims=True)), 1e-12)
    k = k * (1.0 + (a - 1.0) * k_a)
    rh = r.reshape(B_, S_, H, N)
    kh = k.reshape(B_, S_, H, N)
    vh = v.reshape(B_, S_, H, N)
    wh = decay.reshape(B_, S_, H, N)
    ah = a.reshape(B_, S_, H, N)

    def step(state, inp):
        r_t, w_t, k_t, v_t, kk_t, a_t = inp
        sa = jnp.einsum('bhvk,bhk->bhv', state, -kk_t)
        state = (state * w_t[:, :, None, :]
                 + sa[..., None] * (kk_t * a_t)[:, :, None, :]
                 + v_t[..., None] * k_t[:, :, None, :])
        return state, jnp.einsum('bhvk,bhk->bhv', state, r_t)

    xs = (jnp.moveaxis(rh, 1, 0), jnp.moveaxis(wh, 1, 0), jnp.moveaxis(kh, 1, 0),
          jnp.moveaxis(vh, 1, 0), jnp.moveaxis(kk, 1, 0), jnp.moveaxis(ah, 1, 0))
    s0 = jnp.zeros((B_, H, N, N), F32)
    _, y = lax.scan(step, s0, xs)
    y = jnp.moveaxis(y, 0, 1)
    y = head_norm(y, RWKV_GN_EPS) * ln_g.reshape(H, N) + ln_b.reshape(H, N)
    y = y + jnp.sum(rh * kh * r_k, -1, keepdims=True) * vh
    return y.reshape(B_, S_, RWKV_W) * g


def rotary(x, pos):
    d = x.shape[-1]
    half = d // 2
    inv = ROPE_BASE ** (-jnp.arange(half, dtype=F32) / half)
    ang = pos[:, None] * inv[None, :]
    cos, sin = jnp.cos(ang), jnp.sin(ang)
    xf = x.astype(F32)
    x1, x2 = xf[..., :half], xf[..., half:]
    return jnp.concatenate([x1 * cos - x2 * sin, x1 * sin + x2 * cos], axis=-1)


def retention(q, k, v, g, gn_g):
    B_, S_, _ = q.shape
    H, d, C = RET_HEADS, HEAD_DIM, RET_CHUNK
    n_chunk = S_ // C
    to_heads = lambda t: t.reshape(B_, S_, H, d).transpose(0, 2, 1, 3)
    pos = jnp.arange(S_, dtype=F32)
    qh = rotary(to_heads(q), pos)
    kh = rotary(to_heads(k), pos) * (d ** -0.5)
    vh = to_heads(v).astype(F32)
    qc = qh.reshape(B_, H, n_chunk, C, d)
    kc = kh.reshape(B_, H, n_chunk, C, d)
    vc = vh.reshape(B_, H, n_chunk, C, d)
    log_gamma = jnp.log1p(-jnp.exp2(-5.0 - jnp.arange(H, dtype=F32)))
    idx = jnp.arange(C, dtype=F32)
    diff = idx[:, None] - idx[None, :]
    dmask = jnp.where(diff >= 0, jnp.exp(jnp.maximum(diff, 0.0) * log_gamma[:, None, None]), 0.0)
    scores = jnp.einsum('bhncd,bhnsd->bhncs', qc, kc) * dmask[None, :, None]
    y_intra = jnp.einsum('bhncs,bhnse->bhnce', scores, vc)
    k_dec = jnp.exp((C - 1.0 - idx)[None, :] * log_gamma[:, None])
    q_dec = jnp.exp((idx + 1.0)[None, :] * log_gamma[:, None])
    kv = jnp.einsum('bhncd,bhnce->bhnde', kc * k_dec[None, :, None, :, None], vc)
    chunk_decay = jnp.exp(C * log_gamma)[None, :, None, None]

    def step(R, kv_n):
        return R * chunk_decay + kv_n, R

    _, R_prev = lax.scan(step, jnp.zeros((B_, H, d, d), F32), jnp.moveaxis(kv, 2, 0))
    R_prev = jnp.moveaxis(R_prev, 0, 2)
    y_cross = jnp.einsum('bhncd,bhnde->bhnce', qc * q_dec[None, :, None, :, None], R_prev)
    y = (y_intra + y_cross).reshape(B_, H, S_, d).transpose(0, 2, 1, 3)
    y = head_norm(y, RET_GN_EPS) * gn_g.reshape(H, d)
    return y.reshape(B_, S_, RET_W) * jax.nn.silu(g.astype(F32))


def hierarchical_moe(h, w_group, b_group, w_expert, b_expert, w1, w3, w2):
    B_, S_, D = h.shape
    T = B_ * S_
    hf = h.reshape(T, D)
    g_logits = (hf @ w_group + b_group).astype(F32)
    grp = jnp.argmax(g_logits, axis=-1)
    g_gate = jnp.take_along_axis(jax.nn.softmax(g_logits, -1), grp[:, None], axis=-1)
    e_logits = (hf @ w_expert + b_expert).astype(F32).reshape(T, N_GROUPS, EXPERTS_PER_GROUP)
    e_sel = jnp.take_along_axis(e_logits, grp[:, None, None], axis=1)[:, 0]
    top_val, top_idx = lax.top_k(e_sel, TOP_K)
    gates = g_gate * jax.nn.softmax(top_val, axis=-1)
    expert_id = grp[:, None] * EXPERTS_PER_GROUP + top_idx

    A = T * TOP_K
    eid = expert_id.reshape(A)
    tok = jnp.repeat(jnp.arange(T), TOP_K)
    gate = gates.reshape(A)
    order = jnp.argsort(eid)
    e_sorted, tok_sorted, gate_sorted = eid[order], tok[order], gate[order]
    counts = jnp.zeros((N_EXPERTS,), jnp.int32).at[eid].add(1)
    offsets = jnp.cumsum(counts) - counts
    padded = ((counts + MOE_BLOCK - 1) // MOE_BLOCK) * MOE_BLOCK
    pad_end = jnp.cumsum(padded)
    pad_off = pad_end - padded
    dest = pad_off[e_sorted] + (jnp.arange(A) - offsets[e_sorted])
    n_blk = A // MOE_BLOCK + N_EXPERTS
    x_pad = jnp.zeros((n_blk * MOE_BLOCK, D), h.dtype).at[dest].set(hf[tok_sorted])
    block_expert = jnp.minimum(
        jnp.searchsorted(pad_end, jnp.arange(n_blk) * MOE_BLOCK, side='right'), N_EXPERTS - 1)

    def expert_block(args):
        xb, e = args
        return (jax.nn.silu(xb @ w1[e]) * (xb @ w3[e])) @ w2[e]

    y_pad = lax.map(expert_block, (x_pad.reshape(n_blk, MOE_BLOCK, D), block_expert))
    y = y_pad.reshape(n_blk * MOE_BLOCK, D)[dest] * gate_sorted[:, None].astype(h.dtype)
    return jax.ops.segment_sum(y, tok_sorted, num_segments=T).reshape(B_, S_, D)


def setup_inputs(seed: int = 0) -> dict:
    key = jax.random.key(seed)
    ks = iter(jax.random.split(key, 32))
    L, D = DEPTH, D_MODEL

    def nrm(shape, scale):
        return scale * jax.random.normal(next(ks), shape, F32)

    def uni(shape):
        return jax.random.uniform(next(ks), shape, F32)

    return {
        "x": nrm((BATCH, SEQ, D), 1.0),
        "w_in": nrm((L, D, IN_W), D ** -0.5),
        "fox_fgate_bias": jnp.linspace(1.0, 6.0, FOX_HEADS, dtype=F32)[None, :] + nrm((L, FOX_HEADS), 0.1),
        "rwkv_mu_rkv": uni((L, 3, RWKV_W)),
        "rwkv_mu_lora": uni((L, LORA_W)),
        "rwkv_w0": jnp.linspace(-6.0, -1.0, RWKV_W, dtype=F32)[None, :] + nrm((L, RWKV_W), 0.1),
        "rwkv_w2": nrm((L, W_LORA, RWKV_W), 0.5 * W_LORA ** -0.5),
        "rwkv_a0": nrm((L, RWKV_W), 0.1),
        "rwkv_a2": nrm((L, A_LORA, RWKV_W), A_LORA ** -0.5),
        "rwkv_g2": nrm((L, G_LORA, RWKV_W), G_LORA ** -0.5),
        "rwkv_k_k": 0.85 + nrm((L, RWKV_W), 0.05),
        "rwkv_k_a": 1.0 + nrm((L, RWKV_W), 0.05),
        "rwkv_r_k": nrm((L, RWKV_HEADS, HEAD_DIM), 0.1),
        "rwkv_ln_g": 1.0 + nrm((L, RWKV_W), 0.05),
        "rwkv_ln_b": nrm((L, RWKV_W), 0.02),
        "ret_gn_g": 1.0 + nrm((L, RET_W), 0.05),
        "w_out": nrm((L, MIX_W, D), BETA * MIX_W ** -0.5),
        "ln1_g": 1.0 + nrm((L, D), 0.05),
        "ln1_b": nrm((L, D), 0.02),
        "ln2_g": 1.0 + nrm((L, D), 0.05),
        "ln2_b": nrm((L, D), 0.02),
        "moe_w_group": nrm((L, D, N_GROUPS), D ** -0.5),
        "moe_b_group": nrm((L, N_GROUPS), 0.01),
        "moe_w_expert": nrm((L, D, N_EXPERTS), D ** -0.5),
        "moe_b_expert": nrm((L, N_EXPERTS), 0.01),
        "moe_w1": nrm((L, N_EXPERTS, D, D_EXPERT), D ** -0.5),
        "moe_w3": nrm((L, N_EXPERTS, D, D_EXPERT), D ** -0.5),
        "moe_w2": nrm((L, N_EXPERTS, D_EXPERT, D), BETA * D_EXPERT ** -0.5),
    }


def reference(x, w_in, fox_fgate_bias, rwkv_mu_rkv, rwkv_mu_lora, rwkv_w0, rwkv_w2,
              rwkv_a0, rwkv_a2, rwkv_g2, rwkv_k_k, rwkv_k_a, rwkv_r_k, rwkv_ln_g, rwkv_ln_b,
              ret_gn_g, w_out, ln1_g, ln1_b, ln2_g, ln2_b, moe_w_group, moe_b_group,
              moe_w_expert, moe_b_expert, moe_w1, moe_w3, moe_w2):
    B_, S_, D = x.shape
    for l in range(DEPTH):
        p = x @ w_in[l]
        (fq, fk, fv, ff, rr, rk, rv, rwd, rad, rgd, tq, tk, tv, tg) = split_columns(p, IN_SPLITS)
        fox_heads = lambda t: t.reshape(B_, S_, FOX_HEADS, HEAD_DIM)
        y_fox = fox_attention(fox_heads(fq), fox_heads(fk), fox_heads(fv),
                              ff + fox_fgate_bias[l])
        y_rwkv = rwkv7_time_mix(rr, rk, rv, rwd, rad, rgd, rwkv_mu_rkv[l], rwkv_mu_lora[l],
                                rwkv_w0[l], rwkv_w2[l], rwkv_a0[l], rwkv_a2[l], rwkv_g2[l],
                                rwkv_k_k[l], rwkv_k_a[l], rwkv_r_k[l], rwkv_ln_g[l], rwkv_ln_b[l])
        y_ret = retention(tq, tk, tv, tg, ret_gn_g[l])
        mixed = jnp.concatenate([y_fox.astype(x.dtype), y_rwkv.astype(x.dtype),
                                 y_ret.astype(x.dtype)], axis=-1) @ w_out[l]
        x = layer_norm(ALPHA * x + mixed, ln1_g[l], ln1_b[l])
        moe = hierarchical_moe(x, moe_w_group[l], moe_b_group[l], moe_w_expert[l],
                               moe_b_expert[l], moe_w1[l], moe_w3[l], moe_w2[l])
        x = layer_norm(ALPHA * x + moe, ln2_g[l], ln2_b[l])
    return x
```

```python
import bisect
from contextlib import ExitStack
import numpy as np
import concourse.bass as bass
import concourse.mybir as mybir
from concourse.bass_utils import run_bass_kernel_spmd

F32 = mybir.dt.float32
BF16 = mybir.dt.bfloat16
I32 = mybir.dt.int32
AF = mybir.ActivationFunctionType
ALU = mybir.AluOpType
AX = mybir.AxisListType

SEM_LIMIT = 30000
STRICT = True


class Buf:
    __slots__ = ("lw", "rd", "dsem", "dval", "name", "excl")

    def __init__(self, name="", excl=False):
        self.excl = excl
        self.lw = None
        self.rd = []
        self.dsem = None
        self.dval = 0
        self.name = name


class V:
    __slots__ = ("ap", "bufs")

    def __init__(self, ap, bufs):
        self.ap = ap
        self.bufs = bufs

    def __getitem__(self, idx):
        return V(self.ap[idx], self.bufs)

    def with_bufs(self, bufs):
        return V(self.ap, bufs)


class Eng:
    def __init__(self, K, name, h):
        self.K = K
        self.name = name
        self.h = h
        self.n = 0
        self.last = None
        self.sig_idx = []
        self.sig_tok = []
        self.sem = None
        self.semval = 0
        self.known = {}
        self.known_dma = {}

    def signal_for(self, idx):
        p = bisect.bisect_left(self.sig_idx, idx)
        if p < len(self.sig_idx):
            return self.sig_idx[p], self.sig_tok[p]
        assert self.n - 1 >= idx and self.last is not None
        if self.sem is None or self.semval >= SEM_LIMIT:
            self.sem = self.K.new_sem(self.name)
            self.semval = 0
        self.semval += 1
        self.last.then_inc(self.sem, 1)
        self.sig_idx.append(self.n - 1)
        self.sig_tok.append((self.sem, self.semval))
        return self.n - 1, (self.sem, self.semval)


class Kern:
    def __init__(self):
        self.nc = bass.Bass("TRN2", target_bir_lowering=False)
        self.stack = ExitStack()
        nc = self.nc
        self.eng = {
            "pe": Eng(self, "pe", nc.tensor),
            "act": Eng(self, "act", nc.scalar),
            "dve": Eng(self, "dve", nc.vector),
            "pool": Eng(self, "pool", nc.gpsimd),
            "sp": Eng(self, "sp", nc.sync),
        }
        self.nsem = 0
        self.out_toks = []
        self.ninstr = 0

    def new_sem(self, name):
        self.nsem += 1
        return self.stack.enter_context(self.nc.semaphore(f"s{self.nsem}_{name}"))

    def dram(self, name, shape, dtype, kind):
        t = self.nc.dram_tensor(name, list(shape), dtype, kind=kind)
        return V(t.ap(), [Buf(name)])

    def sbuf(self, name, shape, dtype, nbufs=1):
        t = self.stack.enter_context(self.nc.sbuf_tensor(name, list(shape), dtype))
        return V(t[:], [Buf(name)])

    def psum(self, name, shape, dtype=F32):
        t = self.stack.enter_context(self.nc.psum_tensor(name, list(shape), dtype))
        return V(t[:], [Buf(name, excl=True)])

    def _wait_tok(self, F, tok, same_engine_ok):
        if tok is None:
            return
        if tok[0] == "e":
            _, E, idx = tok
            if E is F and same_engine_ok and (F.name == "pe" or not STRICT):
                return
            if F.known.get(E.name, -1) >= idx:
                return
            sidx, (sem, val) = E.signal_for(idx)
            F.h.wait_ge(sem, val)
            F.known[E.name] = sidx
        else:
            _, sem, val, sid = tok
            if F.known_dma.get(sid, 0) >= val:
                return
            F.h.wait_ge(sem, val)
            F.known_dma[sid] = val

    def _deps(self, F, reads, writes):
        for v in reads:
            for b in v.bufs:
                self._wait_tok(F, b.lw, False)
                if b.excl:
                    for t in b.rd:
                        if t[0] == "e" and t[1] is not F:
                            self._wait_tok(F, t, True)
        for v in writes:
            for b in v.bufs:
                self._wait_tok(F, b.lw, True)
                for t in b.rd:
                    self._wait_tok(F, t, True)

    def _commit(self, tok, reads, writes):
        for v in reads:
            for b in v.bufs:
                b.rd.append(tok)
                if len(b.rd) > 24:
                    b.rd = b.rd[-24:] if False else b.rd
        for v in writes:
            for b in v.bufs:
                b.lw = tok
                b.rd = []

    def op(self, ename, fn, reads, writes):
        F = self.eng[ename]
        self._deps(F, reads, writes)
        ins = fn()
        F.last = ins
        idx = F.n
        F.n += 1
        self.ninstr += 1
        self._commit(("e", F, idx), reads, writes)
        return ins

    def dma(self, qname, out, in_, owner=None):
        F = self.eng[qname]
        self._deps(F, [in_], [out])
        if owner is None:
            owner = out.bufs[0]
        if owner.dsem is None:
            owner.dsem = self.new_sem("d")
            owner.dval = 0
        if owner.dval + 16 > SEM_LIMIT * 2:
            raise RuntimeError("dma sem overflow " + str(owner.name))
        owner.dval += 16
        ins = F.h.dma_start(out=out.ap, in_=in_.ap)
        ins.then_inc(owner.dsem, 16)
        self.ninstr += 1
        tok = ("d", owner.dsem, owner.dval, id(owner))
        self._commit(tok, [in_], [out])
        return tok

    def finish(self, toks):
        F = self.eng["sp"]
        for t in toks:
            self._wait_tok(F, t, False)

    def mm(self, out, lhsT, rhs, start=True, stop=True, skip=False):
        return self.op("pe", lambda: self.nc.tensor.matmul(out.ap, lhsT.ap, rhs.ap, start=start, stop=stop,
                                                           skip_group_check=skip),
                       [lhsT, rhs], [out])

    def transpose(self, out, in_, ident):
        return self.op("pe", lambda: self.nc.tensor.transpose(out.ap, in_.ap, ident.ap), [in_, ident], [out])

    def act(self, out, in_, func, bias=None, scale=1.0, accum_out=None):
        reads = [in_]
        kw = {}
        if isinstance(bias, V):
            reads.append(bias); kw["bias"] = bias.ap
        elif bias is not None:
            kw["bias"] = bias
        if isinstance(scale, V):
            reads.append(scale); kw["scale"] = scale.ap
        else:
            kw["scale"] = scale
        writes = [out]
        if accum_out is not None:
            writes.append(accum_out); kw["accum_out"] = accum_out.ap
        return self.op("act", lambda: self.nc.scalar.activation(out=out.ap, in_=in_.ap, func=func, **kw), reads, writes)

    def _e(self, eng):
        return {"dve": self.nc.vector, "pool": self.nc.gpsimd, "act": self.nc.scalar}[eng]

    def copy(self, out, in_, eng="dve"):
        if eng == "act":
            return self.op("act", lambda: self.nc.scalar.copy(out=out.ap, in_=in_.ap), [in_], [out])
        return self.op(eng, lambda: self._e(eng).tensor_copy(out.ap, in_.ap), [in_], [out])

    def tt(self, out, in0, in1, op, eng="dve"):
        return self.op(eng, lambda: self._e(eng).tensor_tensor(out.ap, in0.ap, in1.ap, op), [in0, in1], [out])

    def ts(self, out, in0, s1, op0, s2=None, op1=None, eng="dve", accum_out=None):
        reads = [in0]
        a1 = s1.ap if isinstance(s1, V) else s1
        a2 = s2.ap if isinstance(s2, V) else s2
        if isinstance(s1, V): reads.append(s1)
        if isinstance(s2, V): reads.append(s2)
        writes = [out]
        kw = {}
        if accum_out is not None:
            writes.append(accum_out); kw["accum_out"] = accum_out.ap
        if op1 is None:
            return self.op(eng, lambda: self._e(eng).tensor_scalar(out.ap, in0.ap, a1, None, op0, **kw), reads, writes)
        return self.op(eng, lambda: self._e(eng).tensor_scalar(out.ap, in0.ap, a1, a2, op0, op1, **kw), reads, writes)

    def stt(self, out, in0, s, in1, op0, op1):
        reads = [in0, in1]
        a = s.ap if isinstance(s, V) else s
        if isinstance(s, V): reads.append(s)
        return self.op("dve", lambda: self.nc.vector.scalar_tensor_tensor(out.ap, in0.ap, a, in1.ap, op0, op1), reads, [out])

    def scan(self, out, d0, d1, initial, op0, op1):
        reads = [d0, d1]
        a = initial.ap if isinstance(initial, V) else initial
        if isinstance(initial, V): reads.append(initial)
        return self.op("dve", lambda: self.nc.vector.tensor_tensor_scan(out.ap, d0.ap, d1.ap, a, op0, op1), reads, [out])

    def memset(self, out, val, eng="dve"):
        return self.op(eng, lambda: self._e(eng).memset(out.ap, val), [], [out])

    def recip(self, out, in_):
        return self.op("dve", lambda: self.nc.vector.reciprocal(out.ap, in_.ap), [in_], [out])

    def reduce(self, out, in_, op, axis=AX.X):
        return self.op("dve", lambda: self.nc.vector.tensor_reduce(out.ap, in_.ap, axis, op), [in_], [out])

import math
import numpy as np

G = 512
NT = 4
C = 64
HG = 256
NCH = HG // C
OFF_A, OFF_B, OFF_C, OFF_D, OFF_T1, OFF_MIX = 0, 128, 256, 384, 512, 770
N_MIX = 448
OFF_E, OFF_F, OFF_GD, OFF_T2 = 0, 128, 256, 384
NCOL = OFF_MIX + N_MIX
NCOLB = OFF_MIX + 2 * N_MIX
LWC = 0.6065306597126334


def vr(v, pat, **kw):
    return V(v.ap.rearrange(pat, **kw), v.bufs)


def vb(v, shape):
    return V(v.ap.broadcast_to(list(shape)), v.bufs)


def build_A(S):
    NG = S // G
    NTT = S // 128
    K = Kern()
    xT = K.dram("xT", [1024, S], F32, "ExternalInput")
    W = K.dram("W", [1024, NCOL], F32, "ExternalInput")
    mu = K.dram("mu", [1, N_MIX], F32, "ExternalInput")
    pcol = K.dram("pcol", [64, 8], F32, "ExternalInput")
    w2h = K.dram("w2h", [64, 64], F32, "ExternalInput")
    a2h = K.dram("a2h", [64, 64], F32, "ExternalInput")
    g2h = K.dram("g2h", [128, 64], F32, "ExternalInput")
    prow = K.dram("prow", [1, 194], F32, "ExternalInput")
    cst = K.dram("cst", [128, 512], F32, "ExternalInput")
    cst64 = K.dram("cst64", [64, 192], F32, "ExternalInput")
    rst = K.dram("rst", [64, HG], F32, "ExternalInput")
    rot = K.dram("rot", [64, 2, S], F32, "ExternalInput")
    retc = K.dram("retc", [128, 642], F32, "ExternalInput")
    y = K.dram("y", [S, 256], F32, "ExternalOutput")

    sb = K.sbuf
    wb = sb("wb", [128, 8, NCOLB], BF16)
    stg = [sb(f"stg{i}", [128, 1280], F32) for i in range(2)]
    mub = sb("mub", [128, N_MIX], F32)
    wtmp = sb("wtmp", [128, N_MIX], F32)
    pc = sb("pc", [64, 8], F32)
    w2b = sb("w2b", [64, 64], BF16); a2b = sb("a2b", [64, 64], BF16); g2b = sb("g2b", [128, 64], BF16)
    prb = sb("prb", [128, 194], F32)
    cs = sb("cs", [128, 512], F32)
    cs64 = sb("cs64", [64, 192], F32)
    rstm = sb("rstm", [64, HG], F32)
    rc = sb("rc", [128, 642], F32)
    causal = sb("causal", [128, 128], BF16)
    c8 = sb("c8", [1, 128], BF16)
    ident = cs[:, 0:128]; ucum = cs[:, 128:256]; ones = cs[:, 256:384]
    I64 = cs[0:64, 0:64]

    def m3(i):
        return vb(vr(cs64[:, 64 * i:64 * i + 64], "p (o t) -> p o t", o=1), [64, NCH, C])
    mUs, mLs, mUi = m3(0), m3(1), m3(2)
    I3 = vb(vr(I64, "p (o t) -> p o t", o=1), [64, NCH, C])

    xb = [sb(f"xb{i}", [128, 8, G + 1], BF16) for i in range(2)]
    rott = sb("rott", [64, 2, G], F32)

    Kc = sb("Kc", [128, S], BF16)
    Vc = sb("Vc", [128, NTT, 2, 65], BF16)
    kbufs = [Buf(f"kg{g}") for g in range(NG)]
    vbufs = [Buf(f"vg{g}") for g in range(NG)]
    ctab = sb("ctab", [128, 2, NTT], F32)
    cbufs = [Buf(f"cg{g}") for g in range(NG)]
    carry = [sb(f"carry{i}", [128, 2], F32) for i in range(2)]
    Qa = sb("Qa", [128, G], BF16)
    rqrow = [sb(f"rqrow{h}", [1, G], BF16) for h in range(2)]
    spg = sb("spg", [128, NT, 2], F32)
    spe = sb("spe", [128, NT, 2], F32)
    win = sb("win", [128, 2, NT], F32)
    inc = sb("inc", [128, 2, NT], F32)
    rq = sb("rq", [128, 2, NT], F32)
    biasg = [sb(f"biasg{h}", [128, NTT], F32) for h in range(2)]
    pts = [sb(f"pt{i}", [128, G], BF16) for i in range(3)]
    rcp = sb("rcp", [128, NT], F32)
    yfox = sb("yfox", [128, NT, 128], F32)

    P = [K.psum(f"P{i}", [128, 512], F32) for i in range(8)]

    w2f = stg[0][0:64, 0:64]; a2f = stg[0][0:64, 64:128]; g2f = stg[0][:, 128:192]
    K.dma("sp", pc, pcol)
    K.dma("sp", w2f, w2h); K.dma("sp", a2f, a2h); K.dma("sp", g2f, g2h)
    K.dma("sp", cs, cst); K.dma("sp", cs64, cst64); K.dma("sp", rstm, rst); K.dma("sp", rc, retc)
    K.dma("sp", mub, V(mu.ap.partition_broadcast(128), mu.bufs))
    K.dma("sp", prb, V(prow.ap.partition_broadcast(128), prow.bufs))
    K.copy(w2b, w2f); K.copy(a2b, a2f); K.copy(g2b, g2f)
    K.copy(causal, cs[:, 384:512])
    K.memset(c8, 8.0)
    K.ts(pc[:, 4:5], pc[:, 3:4], -1.0, ALU.mult, 1.0, ALU.add)
    Wv = vr(W, "(kt p) c -> kt p c", p=128)
    for kt in range(8):
        st = stg[kt % 2]
        K.dma("sp", st[:, 0:NCOL], Wv[kt])
        K.copy(wb[:, kt, 0:OFF_MIX], st[:, 0:OFF_MIX], eng="act")
        K.tt(wtmp, st[:, OFF_MIX:NCOL], mub, ALU.mult)
        K.copy(wb[:, kt, OFF_MIX + N_MIX:NCOLB], wtmp, eng="pool")
        K.tt(wb[:, kt, OFF_MIX:OFF_MIX + N_MIX], st[:, OFF_MIX:NCOL], wtmp, ALU.subtract)
    K.memset(Vc[:, :, :, 64:65].with_bufs(vbufs), 1.0)
    K.memset(carry[0], 0.0)
    K.memset(xb[1][:, :, G:G + 1], 0.0)

    xTv = vr(xT, "(kt p) s -> p kt s", p=128)

    def load_x(g, q):
        st = stg[q % 2]
        sv = vr(st[:, 0:1024], "p (k s) -> p k s", k=2)
        K.dma("sp", sv, xTv[:, 2 * q:2 * q + 2, g * G:(g + 1) * G])
        K.copy(xb[g % 2][:, 2 * q:2 * q + 2, 1:G + 1], sv, eng=("act" if q % 2 == 0 else "dve"))

    out_toks = []

    ST = [sb(f"ST{i}", [64, 64], F32) for i in range(2)]
    K.memset(ST[0], 0.0)
    Rst = [sb(f"Rst{i}", [64, 64], F32) for i in range(2)]
    Rb = [sb(f"Rb{i}", [64, 64], BF16) for i in range(2)]
    K.memset(Rst[0], 0.0); K.memset(Rb[0], 0.0)
    st_idx = [0]
    r_idx = [0]

    A = [sb(f"A{i}", [64, HG], F32) for i in range(28)]
    thw = sb("thw", [64, HG], BF16); adb = sb("adb", [64, HG], BF16); sgd = sb("sgd", [128, HG], BF16)
    vtok = sb("vtok", [64, 2 * NCH, 64], F32)
    st1 = sb("st1", [64, NCH], F32); st2 = sb("st2", [64, NCH], F32); rkv = sb("rkv", [64, NCH], F32)
    yrwo = [sb(f"yrwo{i}", [64, NCH, 64], F32) for i in range(2)]
    qrT = sb("qrT", [64, G], BF16); krT = sb("krT", [64, G], BF16); qdT = sb("qdT", [64, G], BF16)
    krTf = sb("krTf", [64, G], F32)
    vret = sb("vret", [128, NT, 64], BF16)
    gret = sb("gret", [128, NT, 64], F32)
    sTr = [sb(f"sTr{i}", [128, 128], BF16) for i in range(2)]
    kdt = [sb(f"kdt{i}", [128, 64], BF16) for i in range(2)]
    yr = sb("yr", [128, NT, 64], F32); yrc = sb("yrc", [128, NT, 64], F32); yrs = sb("yrs", [128, NT, 64], F32)
    rs1 = sb("rs1", [128, NT], F32); rs2 = sb("rs2", [128, NT], F32)
    yreto = sb("yreto", [128, NT, 64], F32)

    def groupnorm(yv, ycv, ysqv, s1, s2, eps, Pn, n):
        K.reduce(s1, yv, ALU.add)
        K.ts(s1, s1, -1.0 / 64, ALU.mult)
        K.tt(ycv, yv, vb(vr(s1, "p (n o) -> p n o", o=1), [Pn, n, 64]), ALU.add)
        K.tt(ysqv, ycv, ycv, ALU.mult)
        K.reduce(s2, ysqv, ALU.add)
        K.ts(s2, s2, 1.0 / 64, ALU.mult, float(eps), ALU.add)
        K.act(s2, s2, AF.Ln)
        K.act(s2, s2, AF.Exp, scale=-0.5)
        K.tt(ycv, ycv, vb(vr(s2, "p (n o) -> p n o", o=1), [Pn, n, 64]), ALU.mult)

    def c3(v):
        return vr(v, "p (c t) -> p c t", c=NCH)

    def ch(v, c):
        return v[:, c * C:(c + 1) * C]

    for q in range(4):
        load_x(0, q)

    for g in range(NG):
        cur, prv = g % 2, (g + 1) % 2
        t0 = g * G
        K.copy(xb[cur][:, :, 0:1], xb[prv][:, :, G:G + 1], eng="pool")
        X = xb[cur]
        K.dma("sp", rott, rot[:, :, t0:t0 + G])

        def proj_fm(pout, off, mixed, c0=0, n=G):
            for kt in range(8):
                if not mixed:
                    K.mm(pout, wb[:, kt, off:off + 128], X[:, kt, 1 + c0:1 + c0 + n], start=(kt == 0), stop=(kt == 7))
                else:
                    K.mm(pout, wb[:, kt, OFF_MIX + off:OFF_MIX + off + 128], X[:, kt, 1 + c0:1 + c0 + n], start=(kt == 0), stop=False)
                    K.mm(pout, wb[:, kt, OFF_MIX + N_MIX + off:OFF_MIX + N_MIX + off + 128], X[:, kt, c0:c0 + n], start=False, stop=(kt == 7))

        proj_fm(P[0], OFF_A, False)
        K.copy(Qa, P[0], eng="act")
        proj_fm(P[1], OFF_B, False)
        K.copy(Kc[:, t0:t0 + G].with_bufs([kbufs[g]]), P[1], eng="dve")
        for tt_ in range(NT):
            pb = tt_ % 2
            tsl = slice(1 + tt_ * 128, 1 + tt_ * 128 + 128)
            tsl_prev = slice(tt_ * 128, tt_ * 128 + 128)
            for kt in range(8):
                K.mm(P[pb][:, 0:258], X[:, kt, tsl], wb[:, kt, OFF_T1:OFF_T1 + 258], start=(kt == 0), stop=(kt == 7))
            for kt in range(8):
                K.mm(P[pb][:, 258:322], X[:, kt, tsl], wb[:, kt, OFF_MIX + OFF_T2:OFF_MIX + OFF_T2 + 64], start=(kt == 0), stop=False)
                K.mm(P[pb][:, 258:322], X[:, kt, tsl_prev], wb[:, kt, OFF_MIX + N_MIX + OFF_T2:OFF_MIX + N_MIX + OFF_T2 + 64], start=False, stop=(kt == 7))
            tile = g * NT + tt_
            K.copy(Vc[:, tile, :, 0:64].with_bufs([vbufs[g]]), vr(P[pb][:, 0:128], "p (h d) -> p h d", h=2), eng="act")
            K.tt(spg[:, tt_, :], P[pb][:, 128:130], prb[:, 0:2], ALU.add)
            K.copy(vret[:, tt_, :], P[pb][:, 130:194], eng="dve")
            K.act(gret[:, tt_, :], P[pb][:, 194:258], AF.Silu)
            for hh in range(2):
                K.copy(vtok[:, tt_ * 2 + hh, :], P[pb][64 * hh:64 * hh + 64, 258:322], eng="dve")
        K.act(spe, spg, AF.Exp, scale=-1.0)
        K.ts(spe, spe, 1.0, ALU.add)
        K.act(spe, spe, AF.Ln)
        spv = vr(spe, "p t h -> p (t h)")
        K.mm(P[0][:, 0:8], ucum, spv)
        K.mm(P[0][:, 8:16], ones, spv)
        pv4 = vr(P[0][:, 0:16], "p (a t h) -> p a h t", a=2, h=2)
        K.copy(win, pv4[:, 0], eng="dve")
        K.copy(rq, pv4[:, 1], eng="dve")
        for h in range(2):
            K.scan(inc[:, h, :], ones[:, 0:NT], rq[:, h, :], carry[cur][:, h:h + 1], ALU.mult, ALU.add)
        K.tt(win, win, inc, ALU.add)
        K.tt(ctab[:, :, g * NT:(g + 1) * NT].with_bufs([cbufs[g]]), win, rq, ALU.subtract)
        K.copy(carry[prv], inc[:, :, NT - 1], eng="dve")
        K.tt(rq, vb(inc[:, :, NT - 1:NT], [128, 2, NT]), inc, ALU.subtract)
        nk = (g + 1) * NT
        for h in range(2):
            K.copy(vr(rqrow[h], "p (j t) -> p j t", j=NT),
                   vb(vr(rq[0:1, h, :], "p (j o) -> p j o", o=1), [1, NT, 128]), eng="dve")
            K.ts(biasg[h][:, 0:nk], ctab[:, h, 0:nk].with_bufs(cbufs[0:g + 1]), inc[:, h, NT - 1:NT], ALU.subtract)
        if g + 1 < NG:
            for q in range(4):
                load_x(g + 1, q)
        pti = 0
        for h in range(2):
            hs = slice(64 * h, 64 * h + 64)
            PV = vr(P[4 + h][:, 0:NT * 65], "p (j e) -> p j e", j=NT)
            for kt in range(nk):
                j = kt - g * NT
                q0 = 128 * max(j, 0)
                ncol = G - q0
                pb = 2 + (kt % 2)
                kg = kt // NT
                K.mm(P[pb][:, 0:ncol], Kc[hs, kt * 128:(kt + 1) * 128].with_bufs([kbufs[kg]]), Qa[hs, q0:G], start=True, stop=False)
                K.mm(P[pb][:, 0:ncol], c8, rqrow[h][:, q0:G], start=False, stop=True)
                pt = pts[pti % 3]; pti += 1
                K.act(pt[:, 0:ncol], P[pb][:, 0:ncol], AF.Exp, bias=biasg[h][:, kt:kt + 1], scale=0.125)
                if j >= 0:
                    K.tt(pt[:, 0:128], pt[:, 0:128], causal, ALU.mult, eng="pool")
                for jj in range(max(j, 0), NT):
                    cc0 = (jj - max(j, 0)) * 128
                    K.mm(PV[:, jj, :], pt[:, cc0:cc0 + 128], Vc[:, kt, h, :].with_bufs([vbufs[kg]]),
                         start=(kt == 0 and jj == 0), stop=(kt == g * NT + jj), skip=True)
            K.recip(rcp, PV[:, :, 64])
            K.tt(yfox[:, :, 64 * h:64 * h + 64], PV[:, :, 0:64], vb(vr(rcp, "p (j o) -> p j o", o=1), [128, NT, 64]), ALU.mult)
        yo = V(y.ap[t0:t0 + G, 0:128].rearrange("(j p) c -> p j c", p=128), [Buf()])
        out_toks.append(K.dma("pool", yo, yfox, owner=yfox.bufs[0]))

        for (off, pbk, dst, dstf) in ((OFF_C, 0, qrT, None), (OFF_D, 1, krT, krTf)):
            proj_fm(P[pbk], off, False)
            for hf in range(2):
                hsl = slice(hf * HG, (hf + 1) * HG)
                K.tt(A[0], P[pbk][0:64, hsl], rott[:, 0, hsl], ALU.mult)
                K.tt(A[1], P[pbk][64:128, hsl], rott[:, 1, hsl], ALU.mult)
                if dstf is None:
                    K.tt(dst[:, hsl], A[0], A[1], ALU.add)
                else:
                    K.tt(dstf[:, hsl], A[0], A[1], ALU.add)
                    K.copy(dst[:, hsl], dstf[:, hsl], eng="act")
        K.tt(qdT, qrT, rc[0:64, 129:641], ALU.mult)
        Yr = vr(P[6][:, 0:NT * 64], "p (j e) -> p j e", j=NT)
        for n in range(NT):
            csl = slice(n * 128, (n + 1) * 128)
            K.mm(P[7][:, 0:128], krT[:, csl], qrT[:, csl])
            sT = sTr[n % 2]
            K.tt(sT, P[7][:, 0:128], rc[:, 0:128], ALU.mult)
            K.transpose(P[7][:, 256:320], krTf[:, csl], I64)
            kd = kdt[n % 2]
            K.ts(kd, P[7][:, 256:320], rc[:, 128:129], ALU.mult)
            ri = r_idx[0]
            K.mm(Yr[:, n, :], sT, vret[:, n, :], start=True, stop=False)
            K.mm(Yr[:, n, :], qdT[:, csl], Rb[ri], start=False, stop=True)
            K.mm(P[7][0:64, 384:448], kd, vret[:, n, :])
            K.stt(Rst[1 - ri], Rst[ri], rc[0:64, 641:642], P[7][0:64, 384:448], ALU.mult, ALU.add)
            K.copy(Rb[1 - ri], Rst[1 - ri], eng="act")
            r_idx[0] = 1 - ri
        K.copy(yr, Yr, eng="act")
        groupnorm(yr, yrc, yrs, rs1, rs2, 1e-6, 128, NT)
        K.tt(yrc, yrc, vb(vr(prb[:, 130:194], "p (o e) -> p o e", o=1), [128, NT, 64]), ALU.mult)
        K.tt(yreto, yrc, gret, ALU.mult)
        yo = V(y.ap[t0:t0 + G, 192:256].rearrange("(j p) c -> p j c", p=128), [Buf()])
        out_toks.append(K.dma("pool", yo, yreto, owner=yreto.bufs[0]))

        for hf in range(2):
            c0 = hf * HG
            (rT, kT, sig, aT, kk, sq, rn, bT, ktl, tmpa, csum, epos, eneg, eprev,
             KpT, BpT, KtT, RpT, BppT, KtppT, Dg, rkr, Pm, PTm, AbrT, AkrT, gtok, spare) = A
            QtT, nLkV, Um, Wm, Gm, HT, yrw = rT, kT, sig, aT, kk, sq, rn
            Kptok, Bpptok, Ktpptok, yc_, ysq, LkTm, MT = bT, ktl, tmpa, csum, epos, eneg, eprev
            p0 = P[0][0:64, 0:HG]; p1 = P[1][0:64, 0:HG]
            proj_fm(P[0][:, 0:HG], OFF_E, True, c0, HG)
            K.copy(rT, P[0][0:64, 0:HG], eng="act")
            K.copy(kT, P[0][64:128, 0:HG], eng="dve")
            proj_fm(P[1][:, 0:HG], OFF_F, True, c0, HG)
            K.act(thw, P[1][0:64, 0:HG], AF.Tanh)
            K.copy(adb, P[1][64:128, 0:HG], eng="dve")
            proj_fm(P[0][:, 0:HG], OFF_GD, True, c0, HG)
            K.act(sgd, P[0][:, 0:HG], AF.Sigmoid)
            K.mm(p1, w2b, thw)
            K.act(sig, p1, AF.Sigmoid, bias=pc[:, 0:1])
            K.mm(p0, a2b, adb)
            K.act(aT, p0, AF.Sigmoid, bias=pc[:, 1:2])
            for t2 in range(2):
                K.mm(P[1][:, 256:320], sgd[:, t2 * 128:(t2 + 1) * 128], g2b)
                for hh in range(2):
                    cch = t2 * 2 + hh
                    K.copy(ch(gtok, cch), P[1][64 * hh:64 * hh + 64, 256:320], eng="act")
            K.ts(kk, kT, pc[:, 2:3], ALU.mult)
            K.tt(sq, kk, kk, ALU.mult)
            K.mm(p0, ones[0:64, 0:64], sq)
            K.act(rn, p0, AF.Sqrt)
            K.ts(rn, rn, 1e-12, ALU.max)
            K.recip(rn, rn)
            K.tt(kk, kk, rn, ALU.mult)
            K.tt(bT, kk, aT, ALU.mult)
            K.ts(tmpa, aT, pc[:, 3:4], ALU.mult, pc[:, 4:5], ALU.add)
            K.tt(ktl, kT, tmpa, ALU.mult)
            K.tt(rkr, rT, ktl, ALU.mult)
            K.ts(rkr, rkr, pc[:, 5:6], ALU.mult)
            K.scan(csum, rstm, sig, 0.0, ALU.mult, ALU.add)
            K.act(epos, csum, AF.Exp, scale=-LWC)
            K.act(eneg, csum, AF.Exp, scale=LWC)
            K.tt(tmpa, csum, sig, ALU.subtract)
            K.act(eprev, tmpa, AF.Exp, scale=-LWC)
            K.tt(KpT, kk, eprev, ALU.mult)
            K.tt(BpT, bT, eneg, ALU.mult)
            K.tt(KtT, ktl, eneg, ALU.mult)
            K.tt(RpT, rT, epos, ALU.mult)
            eCb = vb(c3(epos)[:, :, C - 1:C], [64, NCH, C])
            K.tt(c3(BppT), c3(BpT), eCb, ALU.mult)
            K.tt(c3(KtppT), c3(KtT), eCb, ALU.mult)
            K.tt(c3(Dg), I3, eCb, ALU.mult)

            def pcm(pout, lhs, rhs):
                for c in range(NCH):
                    K.mm(ch(pout, c), ch(lhs, c), ch(rhs, c))
            pcm(p0, BpT, KpT)
            K.stt(c3(PTm), c3(p0), -1.0, mUs, ALU.mult, ALU.mult)
            pcm(p1, KpT, BpT)
            K.stt(c3(Pm), c3(p1), -1.0, mLs, ALU.mult, ALU.mult)
            pcm(p0, KtT, KpT)
            K.tt(c3(LkTm), c3(p0), mUs, ALU.mult)
            pcm(p1, BpT, RpT)
            K.tt(c3(AbrT), c3(p1), mUi, ALU.mult)
            pcm(p0, KtT, RpT)
            K.tt(c3(AkrT), c3(p0), mUi, ALU.mult)
            K.tt(c3(MT), c3(PTm), I3, ALU.add)
            for lvl in range(1, 6):
                pcm(p0, PTm, Pm)
                pcm(p1, Pm, PTm)
                K.copy(Pm, p0, eng="act")
                K.copy(PTm, p1, eng="dve")
                pcm(p0, Pm, MT)
                K.tt(MT, MT, p0, ALU.add)
            for src, dst, pp in ((KpT, Kptok, p0), (BppT, Bpptok, p1), (KtppT, Ktpptok, p0)):
                for c in range(NCH):
                    K.transpose(ch(pp, c), ch(src, c), I64)
                K.copy(dst, pp, eng=("act" if pp is p0 else "dve"))
            vh = vr(vtok[:, hf * NCH:(hf + 1) * NCH, :], "p c e -> p (c e)")
            pcm(p1, LkTm, vh)
            K.ts(nLkV, p1, -1.0, ALU.mult)
            pcm(p0, MT, nLkV)
            K.copy(Um, p0, eng="act")
            pcm(p1, MT, Kptok)
            K.copy(Wm, p1, eng="dve")
            pcm(p0, Wm, Bpptok)
            K.tt(Gm, Dg, p0, ALU.subtract)
            for c in range(NCH):
                K.mm(ch(p1, c), ch(Bpptok, c), ch(Um, c), start=True, stop=False)
                K.mm(ch(p1, c), ch(Ktpptok, c), ch(vh, c), start=False, stop=True)
            K.copy(HT, p1, eng="act")
            pcm(p0, Wm, AbrT)
            K.tt(QtT, RpT, p0, ALU.subtract)
            for c in range(NCH):
                K.mm(P[1][0:64, 448 + c:449 + c], ch(rkr, c), ones[0:64, 0:1])
            K.copy(rkv, P[1][0:64, 448:448 + NCH], eng="dve")
            Yp = P[6][0:64, 256:256 + HG]
            for c in range(NCH):
                si = st_idx[0]
                K.mm(ch(Yp, c), ch(QtT, c), ST[si], start=True, stop=False)
                K.mm(ch(Yp, c), ch(AbrT, c), ch(Um, c), start=False, stop=False)
                K.mm(ch(Yp, c), ch(AkrT, c), ch(vh, c), start=False, stop=True)
                K.mm(P[7][0:64, 448:512], ch(Gm, c), ST[si])
                K.tt(ST[1 - si], P[7][0:64, 448:512], ch(HT, c), ALU.add)
                st_idx[0] = 1 - si
            K.copy(yrw, Yp, eng="act")
            groupnorm(c3(yrw), c3(yc_), c3(ysq), st1, st2, 64e-5, 64, NCH)
            K.tt(c3(yc_), c3(yc_), vb(vr(prb[0:64, 2:66], "p (o e) -> p o e", o=1), [64, NCH, 64]), ALU.mult)
            K.tt(c3(yc_), c3(yc_), vb(vr(prb[0:64, 66:130], "p (o e) -> p o e", o=1), [64, NCH, 64]), ALU.add)
            K.tt(c3(ysq), c3(vh), vb(vr(rkv, "p (c o) -> p c o", o=1), [64, NCH, 64]), ALU.mult)
            K.tt(yc_, yc_, ysq, ALU.add)
            yo_t = yrwo[hf]
            K.tt(vr(yo_t, "p c e -> p (c e)"), yc_, gtok, ALU.mult)
            yo = V(y.ap[t0 + c0:t0 + c0 + HG, 128:192].rearrange("(c p) e -> p c e", p=C), [Buf()])
            out_toks.append(K.dma("pool", yo, yo_t, owner=yo_t.bufs[0]))

    K.finish(out_toks)
    return K


def host_consts(S, head):
    ident = np.eye(128, dtype=np.float32)
    ucum = np.triu(np.ones((128, 128), np.float32))
    ones = np.ones((128, 128), np.float32)
    causal = np.triu(np.ones((128, 128), np.float32))
    cst = np.concatenate([ident, ucum, ones, causal], 1)
    mUs = np.triu(np.ones((64, 64), np.float32), 1)
    mLs = np.tril(np.ones((64, 64), np.float32), -1)
    mUi = np.triu(np.ones((64, 64), np.float32), 0)
    cst64 = np.concatenate([mUs, mLs, mUi], 1)
    rst = np.ones((64, HG), np.float32); rst[:, ::64] = 0.0
    half = 32
    inv = 10000.0 ** (-np.arange(half, dtype=np.float64) / half)
    inv = inv.astype(np.float32).astype(np.float64)
    pos = np.arange(S, dtype=np.float64)
    ang = (pos[None, :].astype(np.float32) * inv[:, None].astype(np.float32)).astype(np.float64)
    cos, sin = np.cos(ang), np.sin(ang)
    CC = np.concatenate([cos, cos], 0)
    SS = np.concatenate([-sin, sin], 0)
    rot = np.ascontiguousarray(np.stack([CC, SS], 1).astype(np.float32))
    lg = np.log1p(-np.exp2(-5.0 - head))
    idx = np.arange(128, dtype=np.float64)
    diff = idx[None, :] - idx[:, None]
    dmT = np.where(diff >= 0, np.exp(np.maximum(diff, 0) * lg), 0.0) * 0.125
    kdec = np.exp((127.0 - idx) * lg) * 0.125
    qdec = np.exp((idx + 1.0) * lg)
    retc = np.zeros((128, 642), np.float32)
    retc[:, 0:128] = dmT
    retc[:, 128] = kdec
    retc[:, 129:641] = np.tile(qdec, 4)[None, :]
    retc[:, 641] = np.exp(128 * lg)
    return dict(cst=cst, cst64=cst64, rst=rst, rot=rot, retc=retc)


IN_SPL = (512, 512, 512, 8, 256, 256, 256, 64, 64, 128, 256, 256, 256, 256)
IN_OFF = np.concatenate([[0], np.cumsum(IN_SPL)])


def host_layer_inputs(inp, l, j):
    w_in = np.asarray(inp["w_in"][l])
    o = IN_OFF
    fq, fk, fv, ff, rr, rk, rv, rwd, rad, rgd, tq, tk, tv, tg = [w_in[:, o[i]:o[i + 1]] for i in range(14)]
    h0, h1 = 2 * j, 2 * j + 1
    sl = lambda w, h: w[:, 64 * h:64 * h + 64]
    sw = lambda w: np.concatenate([w[:, 32:64], w[:, 0:32]], 1)
    cols = [sl(fq, h0), sl(fq, h1), sl(fk, h0), sl(fk, h1),
            sl(tq, j), sw(sl(tq, j)), sl(tk, j), sw(sl(tk, j)),
            sl(fv, h0), sl(fv, h1), ff[:, h0:h0 + 2], sl(tv, j), sl(tg, j),
            sl(rr, j), sl(rk, j), rwd, rad, rgd, sl(rv, j)]
    W = np.ascontiguousarray(np.concatenate(cols, 1), dtype=np.float32)
    assert W.shape[1] == NCOL
    mr = np.asarray(inp["rwkv_mu_rkv"][l]); ml = np.asarray(inp["rwkv_mu_lora"][l])
    c64 = slice(64 * j, 64 * j + 64)
    mu = np.concatenate([mr[0, c64], mr[1, c64], ml, mr[2, c64]])[None, :].astype(np.float32)
    pcol = np.zeros((64, 8), np.float32)
    pcol[:, 0] = np.asarray(inp["rwkv_w0"][l])[c64]
    pcol[:, 1] = np.asarray(inp["rwkv_a0"][l])[c64]
    pcol[:, 2] = np.asarray(inp["rwkv_k_k"][l])[c64]
    pcol[:, 3] = np.asarray(inp["rwkv_k_a"][l])[c64]
    pcol[:, 5] = np.asarray(inp["rwkv_r_k"][l])[j]
    prow = np.concatenate([np.asarray(inp["fox_fgate_bias"][l])[h0:h0 + 2],
                           np.asarray(inp["rwkv_ln_g"][l])[c64], np.asarray(inp["rwkv_ln_b"][l])[c64],
                           np.asarray(inp["ret_gn_g"][l])[c64]])[None, :].astype(np.float32)
    return dict(W=W, mu=mu, pcol=pcol, prow=prow,
                w2h=np.ascontiguousarray(np.asarray(inp["rwkv_w2"][l])[:, c64]),
                a2h=np.ascontiguousarray(np.asarray(inp["rwkv_a2"][l])[:, c64]),
                g2h=np.ascontiguousarray(np.asarray(inp["rwkv_g2"][l])[:, c64]))

import numpy as np

ALPHA = (2.0 * 4) ** 0.25
LN_EPS = 1e-5
NE = 32


def build_B(NTOK, GN):
    NGR = NTOK // GN
    NTL = GN // 128
    HB = min(512, GN)
    NHB = GN // HB
    K = Kern()
    ycT = K.dram("ycT", [1024, NTOK], F32, "ExternalInput")
    xin = K.dram("xin", [NTOK, 1024], F32, "ExternalInput")
    wout = K.dram("wout", [1024, 1024], F32, "ExternalInput")
    lnp = K.dram("lnp", [1, 4096], F32, "ExternalInput")
    wr = K.dram("wr", [1024, 36], F32, "ExternalInput")
    br = K.dram("br", [1, 36], F32, "ExternalInput")
    w1 = K.dram("w1", [NE, 1024, 512], F32, "ExternalInput")
    w3 = K.dram("w3", [NE, 1024, 512], F32, "ExternalInput")
    w2 = K.dram("w2", [NE, 512, 1024], F32, "ExternalInput")
    idn = K.dram("idn", [128, 128], F32, "ExternalInput")
    out = K.dram("out", [NTOK, 1024], F32, "ExternalOutput")

    sb = K.sbuf
    ident = sb("ident", [128, 128], F32)
    lnb = sb("lnb", [128, 4096], F32)
    wrb = sb("wrb", [128, 8, 36], F32)
    brb = sb("brb", [128, 36], F32)
    woutb = sb("woutb", [128, 8, 1024], BF16)
    stg = [sb(f"stg{i}", [128, 2048], F32) for i in range(2)]
    w1b = [sb(f"w1b{i}", [128, 8, 512], BF16) for i in range(2)]
    w3b = [sb(f"w3b{i}", [128, 8, 512], BF16) for i in range(2)]
    w2b = [sb(f"w2b{i}", [128, 4, 1024], BF16) for i in range(2)]
    x1 = sb("x1", [128, NTL, 1024], F32)
    yacc = sb("yacc", [128, NTL, 1024], F32)
    x1Tb = sb("x1Tb", [128, 8, GN], BF16)
    x1Tf = sb("x1Tf", [128, 8, 128], F32)
    ycb = sb("ycb", [128, 8, 128], BF16)
    hb = sb("hb", [128, 1024], F32)
    hc = sb("hc", [128, 1024], F32)
    gates = sb("gates", [128, NTL, NE], F32)
    lg = sb("lg", [128, 36], F32)
    sm = [sb(f"sm{i}", [128, 8], F32) for i in range(10)]
    mg = sb("mg", [128, 4], F32)
    esel = sb("esel", [128, 8], F32); e2 = sb("e2", [128, 8], F32)
    tmp32 = sb("tmp32", [128, 32], F32)
    ge = sb("ge", [128, 8], F32)
    st = [sb(f"lnst{i}", [128, 1], F32) for i in range(4)]
    sil = [sb(f"sil{i}", [128, HB], BF16) for i in range(2)]
    hg = [sb(f"hg{i}", [128, NHB, HB], BF16) for i in range(4)]

    P = [K.psum(f"P{i}", [128, 512], F32) for i in range(8)]

    K.dma("sp", ident, idn)
    K.dma("sp", lnb, V(lnp.ap.partition_broadcast(128), lnp.bufs))
    K.dma("sp", brb, V(br.ap.partition_broadcast(128), br.bufs))
    K.dma("sp", wrb, vr(wr, "(kt p) c -> p kt c", p=128))
    wov = vr(wout, "(kt p) c -> p kt c", p=128)
    for q in range(4):
        sv = vr(stg[q % 2], "p (k c) -> p k c", k=2)
        K.dma("sp", sv, wov[:, 2 * q:2 * q + 2, :])
        K.copy(woutb[:, 2 * q:2 * q + 2, :], sv, eng=("act" if q % 2 == 0 else "dve"))

    def layernorm(dst, src, goff, boff):
        K.reduce(st[0], src, ALU.add)
        K.ts(st[0], st[0], -1.0 / 1024, ALU.mult)
        K.ts(hc, src, st[0], ALU.add)
        K.act(dst, hc, AF.Square, accum_out=st[1])
        K.ts(st[1], st[1], 1.0 / 1024, ALU.mult, LN_EPS, ALU.add)
        K.act(st[1], st[1], AF.Ln)
        K.act(st[1], st[1], AF.Exp, scale=-0.5)
        K.ts(hc, hc, st[1], ALU.mult)
        K.tt(hc, hc, lnb[:, goff:goff + 1024], ALU.mult)
        K.tt(dst, hc, lnb[:, boff:boff + 1024], ALU.add)

    ycTv = vr(ycT, "(kt p) s -> p kt s", p=128)
    xinv = vr(xin, "(n p) d -> n p d", p=128)
    outv = vr(out, "(n p) d -> n p d", p=128)
    w1v = vr(w1, "e (kt p) c -> e p kt c", p=128)
    w3v = vr(w3, "e (kt p) c -> e p kt c", p=128)
    w2v = vr(w2, "e (kt p) c -> e p kt c", p=128)
    out_toks = []
    wcount = [0]

    for gr in range(NGR):
        for tl in range(NTL):
            tile = gr * NTL + tl
            tok0 = tile * 128
            sv = vr(stg[0][:, 0:1024], "p (k c) -> p k c", k=8)
            K.dma("sp", sv, ycTv[:, :, tok0:tok0 + 128])
            K.copy(ycb, sv, eng="act")
            K.dma("sp", stg[1][:, 0:1024], xinv[tile])
            for half in range(2):
                for kt in range(8):
                    K.mm(P[half], ycb[:, kt, :], woutb[:, kt, 512 * half:512 * half + 512], start=(kt == 0), stop=(kt == 7))
            for half in range(2):
                K.stt(hb[:, 512 * half:512 * half + 512], stg[1][:, 512 * half:512 * half + 512], ALPHA, P[half], ALU.mult, ALU.add)
            layernorm(x1[:, tl, :], hb, 0, 1024)
            for half in range(2):
                for k4 in range(4):
                    kt = half * 4 + k4
                    K.transpose(P[2 + half][:, k4 * 128:(k4 + 1) * 128], x1[:, tl, kt * 128:(kt + 1) * 128], ident)
                K.copy(vr(x1Tf[:, 4 * half:4 * half + 4, :], "p k t -> p (k t)"), P[2 + half], eng=("act" if half == 0 else "dve"))
            K.copy(x1Tb[:, :, tl * 128:(tl + 1) * 128], x1Tf, eng="pool")
            for kt in range(8):
                K.mm(P[4][:, 0:36], x1Tf[:, kt, :], wrb[:, kt, :], start=(kt == 0), stop=(kt == 7))
            K.tt(lg, P[4][:, 0:36], brb, ALU.add)
            gl = lg[:, 0:4]
            gmax, gsum, m1, m2, wa, wb_, gg = sm[0][:, 0:1], sm[1][:, 0:1], sm[2][:, 0:1], sm[3][:, 0:1], sm[4][:, 0:1], sm[5][:, 0:1], sm[6][:, 0:1]
            K.reduce(gmax, gl, ALU.max)
            K.ts(mg, gl, gmax, ALU.is_equal)
            K.ts(sm[7][:, 0:4], gl, gmax, ALU.subtract)
            K.act(sm[7][:, 0:4], sm[7][:, 0:4], AF.Exp, accum_out=gsum)
            K.recip(gg, gsum)
            el = vr(lg[:, 4:36], "p (g e) -> p g e", g=4)
            K.tt(vr(tmp32, "p (g e) -> p g e", g=4), el, vb(vr(mg, "p (g o) -> p g o", o=1), [128, 4, 8]), ALU.mult)
            K.reduce(esel, vr(tmp32, "p (g e) -> p e g", g=4), ALU.add)
            K.reduce(m1, esel, ALU.max)
            K.ts(sm[8], esel, m1, ALU.is_equal)
            K.stt(e2, sm[8], -1e30, esel, ALU.mult, ALU.add)
            K.reduce(m2, e2, ALU.max)
            K.ts(sm[9], e2, m2, ALU.is_equal)
            K.tt(wa, m2, m1, ALU.subtract)
            K.act(wa, wa, AF.Exp)
            K.ts(wa, wa, 1.0, ALU.add)
            K.recip(wa, wa)
            K.ts(wb_, wa, -1.0, ALU.mult, 1.0, ALU.add)
            K.tt(wa, wa, gg, ALU.mult)
            K.tt(wb_, wb_, gg, ALU.mult)
            K.ts(ge, sm[8], wa, ALU.mult)
            K.stt(ge, sm[9], wb_, ge, ALU.mult, ALU.add)
            K.tt(vr(gates[:, tl, :], "p (g e) -> p g e", g=4), vb(vr(mg, "p (g o) -> p g o", o=1), [128, 4, 8]),
                 vb(vr(ge, "p (o e) -> p o e", o=1), [128, 4, 8]), ALU.mult)
        for e in range(NE):
            wi = wcount[0] % 2; wcount[0] += 1
            for (src, dstb, k4n, eng) in ((w1v, w1b, 8, "act"), (w3v, w3b, 8, "dve")):
                for hq in range(2):
                    s_ = stg[hq]
                    sv = vr(s_[:, 0:2048], "p (k c) -> p k c", k=4)
                    K.dma("sp", sv, src[e][:, 4 * hq:4 * hq + 4, :])
                    K.copy(dstb[wi][:, 4 * hq:4 * hq + 4, :], sv, eng=(eng if hq == 0 else "pool"))
            for hq in range(2):
                sv = vr(stg[hq][:, 0:2048], "p (k c) -> p k c", k=2)
                K.dma("sp", sv, w2v[e][:, 2 * hq:2 * hq + 2, :])
                K.copy(w2b[wi][:, 2 * hq:2 * hq + 2, :], sv, eng=("act" if hq == 0 else "dve"))
            for hbk in range(NHB):
                cs_ = slice(hbk * HB, (hbk + 1) * HB)
                for nt in range(4):
                    pa, pb = P[nt % 2], P[2 + nt % 2]
                    for kt in range(8):
                        K.mm(pa[:, 0:HB], w1b[wi][:, kt, nt * 128:(nt + 1) * 128], x1Tb[:, kt, cs_], start=(kt == 0), stop=(kt == 7))
                    for kt in range(8):
                        K.mm(pb[:, 0:HB], w3b[wi][:, kt, nt * 128:(nt + 1) * 128], x1Tb[:, kt, cs_], start=(kt == 0), stop=(kt == 7))
                    s2 = sil[nt % 2]
                    K.act(s2, pa[:, 0:HB], AF.Silu)
                    K.tt(hg[nt][:, hbk, :], s2, pb[:, 0:HB], ALU.mult)
            for tl in range(NTL):
                hbk, off = (tl * 128) // HB, (tl * 128) % HB
                for half in range(2):
                    py = P[4 + 2 * (tl % 2) + half]
                    for kt in range(4):
                        K.mm(py, hg[kt][:, hbk, off:off + 128], w2b[wi][:, kt, 512 * half:512 * half + 512], start=(kt == 0), stop=(kt == 3))
                    ysl = yacc[:, tl, 512 * half:512 * half + 512]
                    if e == 0:
                        K.ts(ysl, py, gates[:, tl, e:e + 1], ALU.mult)
                    else:
                        K.stt(ysl, py, gates[:, tl, e:e + 1], ysl, ALU.mult, ALU.add)
        for tl in range(NTL):
            tile = gr * NTL + tl
            K.stt(hb, x1[:, tl, :], ALPHA, yacc[:, tl, :], ALU.mult, ALU.add)
            layernorm(yacc[:, tl, :], hb, 2048, 3072)
            yo = V(outv.ap[tile], [Buf()])
            out_toks.append(K.dma("pool", yo, yacc[:, tl, :], owner=yacc.bufs[0]))
    K.finish(out_toks)
    return K


_CACHE = {}


def _get_A(S):
    if ("A", S) not in _CACHE:
        _CACHE[("A", S)] = build_A(S)
    return _CACHE[("A", S)]


def _get_B(NTOK, GN):
    if ("B", NTOK, GN) not in _CACHE:
        _CACHE[("B", NTOK, GN)] = build_B(NTOK, GN)
    return _CACHE[("B", NTOK, GN)]


def host_B(inp, l):
    return dict(wout=np.ascontiguousarray(inp["w_out"][l]),
                lnp=np.concatenate([inp["ln1_g"][l], inp["ln1_b"][l], inp["ln2_g"][l], inp["ln2_b"][l]])[None, :].astype(np.float32),
                wr=np.ascontiguousarray(np.concatenate([inp["moe_w_group"][l], inp["moe_w_expert"][l]], 1)),
                br=np.concatenate([inp["moe_b_group"][l], inp["moe_b_expert"][l]])[None, :].astype(np.float32),
                w1=np.ascontiguousarray(inp["moe_w1"][l]), w3=np.ascontiguousarray(inp["moe_w3"][l]),
                w2=np.ascontiguousarray(inp["moe_w2"][l]),
                idn=np.eye(128, dtype=np.float32))


def kernel(**inputs):
    inp = {k: np.asarray(v) for k, v in inputs.items()}
    x = np.ascontiguousarray(inp["x"], dtype=np.float32)
    B_, S_, D = x.shape
    NC = 8
    T = B_ * S_
    NTOK = T // NC
    depth = inp["w_in"].shape[0]
    consts = [host_consts(S_, j) for j in range(4)]
    for l in range(depth):
        KA = _get_A(S_)
        xTs = [np.ascontiguousarray(x[b].T) for b in range(B_)]
        in_maps = []
        for c in range(NC):
            b, j = c // 4, c % 4
            d = dict(xT=xTs[b])
            d.update(consts[j])
            d.update(host_layer_inputs(inp, l, j))
            in_maps.append(d)
        res = run_bass_kernel_spmd(KA.nc, in_maps, core_ids=list(range(NC)), trace=True)
        ycat = np.empty((B_, S_, D), np.float32)
        for c in range(NC):
            b, j = c // 4, c % 4
            yk = res.results[c]["y"]
            ycat[b, :, 128 * j:128 * j + 128] = yk[:, 0:128]
            ycat[b, :, 512 + 64 * j:512 + 64 * j + 64] = yk[:, 128:192]
            ycat[b, :, 768 + 64 * j:768 + 64 * j + 64] = yk[:, 192:256]
        KB = _get_B(NTOK, 1024)
        hb = host_B(inp, l)
        yf = ycat.reshape(T, D)
        xf = x.reshape(T, D)
        in_maps = []
        for c in range(NC):
            sl = slice(c * NTOK, (c + 1) * NTOK)
            d = dict(ycT=np.ascontiguousarray(yf[sl].T), xin=np.ascontiguousarray(xf[sl]))
            d.update(hb)
            in_maps.append(d)
        res = run_bass_kernel_spmd(KB.nc, in_maps, core_ids=list(range(NC)), trace=True)
        x = np.concatenate([res.results[c]["out"] for c in range(NC)], 0).reshape(B_, S_, D)
    return x.astype(np.float32)
```

```python
import bisect
from contextlib import ExitStack
import numpy as np
import concourse.bass as bass
import concourse.mybir as mybir
from concourse.bass_utils import run_bass_kernel_spmd

F32 = mybir.dt.float32
BF16 = mybir.dt.bfloat16
I32 = mybir.dt.int32
AF = mybir.ActivationFunctionType
ALU = mybir.AluOpType
AX = mybir.AxisListType

SEM_LIMIT = 30000
STRICT = True


class Buf:
    __slots__ = ("lw", "rd", "dsem", "dval", "name", "excl")

    def __init__(self, name="", excl=False):
        self.excl = excl
        self.lw = None
        self.rd = []
        self.dsem = None
        self.dval = 0
        self.name = name


class V:
    __slots__ = ("ap", "bufs")

    def __init__(self, ap, bufs):
        self.ap = ap
        self.bufs = bufs

    def __getitem__(self, idx):
        return V(self.ap[idx], self.bufs)

    def with_bufs(self, bufs):
        return V(self.ap, bufs)


class Eng:
    def __init__(self, K, name, h):
        self.K = K
        self.name = name
        self.h = h
        self.n = 0
        self.last = None
        self.sig_idx = []
        self.sig_tok = []
        self.sem = None
        self.semval = 0
        self.known = {}
        self.known_dma = {}

    def signal_for(self, idx):
        p = bisect.bisect_left(self.sig_idx, idx)
        if p < len(self.sig_idx):
            return self.sig_idx[p], self.sig_tok[p]
        assert self.n - 1 >= idx and self.last is not None
        if self.sem is None or self.semval >= SEM_LIMIT:
            self.sem = self.K.new_sem(self.name)
            self.semval = 0
        self.semval += 1
        self.last.then_inc(self.sem, 1)
        self.sig_idx.append(self.n - 1)
        self.sig_tok.append((self.sem, self.semval))
        return self.n - 1, (self.sem, self.semval)


class Kern:
    def __init__(self, num_devices=None):
        if num_devices is None:
            self.nc = bass.Bass("TRN2", target_bir_lowering=False)
        else:
            self.nc = bass.Bass("TRN2", target_bir_lowering=False, num_devices=num_devices)
        self.stack = ExitStack()
        nc = self.nc
        self.eng = {
            "pe": Eng(self, "pe", nc.tensor),
            "act": Eng(self, "act", nc.scalar),
            "dve": Eng(self, "dve", nc.vector),
            "pool": Eng(self, "pool", nc.gpsimd),
            "sp": Eng(self, "sp", nc.sync),
        }
        self.nsem = 0
        self.out_toks = []
        self.ninstr = 0
        self.defer = None

    def new_sem(self, name):
        self.nsem += 1
        return self.stack.enter_context(self.nc.semaphore(f"s{self.nsem}_{name}"))

    def dram(self, name, shape, dtype, kind):
        t = self.nc.dram_tensor(name, list(shape), dtype, kind=kind)
        return V(t.ap(), [Buf(name)])

    def sbuf(self, name, shape, dtype, nbufs=1):
        t = self.stack.enter_context(self.nc.sbuf_tensor(name, list(shape), dtype))
        return V(t[:], [Buf(name)])

    def psum(self, name, shape, dtype=F32):
        t = self.stack.enter_context(self.nc.psum_tensor(name, list(shape), dtype))
        return V(t[:], [Buf(name, excl=True)])

    def _wait_tok(self, F, tok, same_engine_ok):
        if tok is None:
            return
        if tok[0] == "e":
            _, E, idx = tok
            if E is F and same_engine_ok and (F.name == "pe" or not STRICT):
                return
            if F.known.get(E.name, -1) >= idx:
                return
            sidx, (sem, val) = E.signal_for(idx)
            F.h.wait_ge(sem, val)
            F.known[E.name] = sidx
        else:
            _, sem, val, sid = tok
            if F.known_dma.get(sid, 0) >= val:
                return
            F.h.wait_ge(sem, val)
            F.known_dma[sid] = val

    def _deps(self, F, reads, writes):
        for v in reads:
            for b in v.bufs:
                self._wait_tok(F, b.lw, False)
                if b.excl:
                    for t in b.rd:
                        if t[0] == "e" and t[1] is not F:
                            self._wait_tok(F, t, True)
        for v in writes:
            for b in v.bufs:
                self._wait_tok(F, b.lw, True)
                for t in b.rd:
                    self._wait_tok(F, t, True)

    def _commit(self, tok, reads, writes):
        for v in reads:
            for b in v.bufs:
                b.rd.append(tok)
                if len(b.rd) > 24:
                    b.rd = b.rd[-24:] if False else b.rd
        for v in writes:
            for b in v.bufs:
                b.lw = tok
                b.rd = []

    def op(self, ename, fn, reads, writes):
        if self.defer is not None:
            self.defer.append(("op", ename, fn, reads, writes))
            return None
        F = self.eng[ename]
        self._deps(F, reads, writes)
        ins = fn()
        F.last = ins
        idx = F.n
        F.n += 1
        self.ninstr += 1
        self._commit(("e", F, idx), reads, writes)
        return ins

    def replay(self, items):
        for it in items:
            if it[0] == "op":
                self.op(it[1], it[2], it[3], it[4])
            else:
                tok = self.dma(it[1], it[2], it[3], owner=it[4])
                if it[5] is not None:
                    it[5](tok)

    def dma(self, qname, out, in_, owner=None, cb=None):
        if self.defer is not None:
            self.defer.append(("dma", qname, out, in_, owner, cb))
            return None
        F = self.eng[qname]
        self._deps(F, [in_], [out])
        if owner is None:
            owner = out.bufs[0]
        if owner.dsem is None:
            owner.dsem = self.new_sem("d")
            owner.dval = 0
        if owner.dval + 16 > SEM_LIMIT * 2:
            raise RuntimeError("dma sem overflow " + str(owner.name))
        owner.dval += 16
        ins = F.h.dma_start(out=out.ap, in_=in_.ap)
        ins.then_inc(owner.dsem, 16)
        self.ninstr += 1
        tok = ("d", owner.dsem, owner.dval, id(owner))
        self._commit(tok, [in_], [out])
        return tok

    def allgather(self, out, in_, ranks):
        F = self.eng["pool"]
        self._deps(F, [in_], [out])
        owner = out.bufs[0]
        if owner.dsem is None:
            owner.dsem = self.new_sem("cc")
            owner.dval = 0
        owner.dval += 16
        ins = self.nc.gpsimd.collective_compute("AllGather", ALU.bypass, replica_groups=[list(range(ranks))],
                                                ins=[in_.ap], outs=[out.ap])
        ins.then_inc(owner.dsem, 16)
        self.ninstr += 1
        tok = ("d", owner.dsem, owner.dval, id(owner))
        self._commit(tok, [in_], [out])
        return tok

    def finish(self, toks):
        F = self.eng["sp"]
        for t in toks:
            self._wait_tok(F, t, False)

    def mm(self, out, lhsT, rhs, start=True, stop=True, skip=False):
        return self.op("pe", lambda: self.nc.tensor.matmul(out.ap, lhsT.ap, rhs.ap, start=start, stop=stop,
                                                           skip_group_check=skip),
                       [lhsT, rhs], [out])

    def transpose(self, out, in_, ident):
        return self.op("pe", lambda: self.nc.tensor.transpose(out.ap, in_.ap, ident.ap), [in_, ident], [out])

    def act(self, out, in_, func, bias=None, scale=1.0, accum_out=None):
        reads = [in_]
        kw = {}
        if isinstance(bias, V):
            reads.append(bias); kw["bias"] = bias.ap
        elif bias is not None:
            kw["bias"] = bias
        if isinstance(scale, V):
            reads.append(scale); kw["scale"] = scale.ap
        else:
            kw["scale"] = scale
        writes = [out]
        if accum_out is not None:
            writes.append(accum_out); kw["accum_out"] = accum_out.ap
        return self.op("act", lambda: self.nc.scalar.activation(out=out.ap, in_=in_.ap, func=func, **kw), reads, writes)

    def _e(self, eng):
        return {"dve": self.nc.vector, "pool": self.nc.gpsimd, "act": self.nc.scalar}[eng]

    def copy(self, out, in_, eng="dve"):
        if eng == "act":
            return self.op("act", lambda: self.nc.scalar.copy(out=out.ap, in_=in_.ap), [in_], [out])
        return self.op(eng, lambda: self._e(eng).tensor_copy(out.ap, in_.ap), [in_], [out])

    def tt(self, out, in0, in1, op, eng="dve"):
        return self.op(eng, lambda: self._e(eng).tensor_tensor(out.ap, in0.ap, in1.ap, op), [in0, in1], [out])

    def ts(self, out, in0, s1, op0, s2=None, op1=None, eng="dve", accum_out=None):
        reads = [in0]
        a1 = s1.ap if isinstance(s1, V) else s1
        a2 = s2.ap if isinstance(s2, V) else s2
        if isinstance(s1, V): reads.append(s1)
        if isinstance(s2, V): reads.append(s2)
        writes = [out]
        kw = {}
        if accum_out is not None:
            writes.append(accum_out); kw["accum_out"] = accum_out.ap
        if op1 is None:
            return self.op(eng, lambda: self._e(eng).tensor_scalar(out.ap, in0.ap, a1, None, op0, **kw), reads, writes)
        return self.op(eng, lambda: self._e(eng).tensor_scalar(out.ap, in0.ap, a1, a2, op0, op1, **kw), reads, writes)

    def stt(self, out, in0, s, in1, op0, op1):
        reads = [in0, in1]
        a = s.ap if isinstance(s, V) else s
        if isinstance(s, V): reads.append(s)
        return self.op("dve", lambda: self.nc.vector.scalar_tensor_tensor(out.ap, in0.ap, a, in1.ap, op0, op1), reads, [out])

    def scan(self, out, d0, d1, initial, op0, op1):
        reads = [d0, d1]
        a = initial.ap if isinstance(initial, V) else initial
        if isinstance(initial, V): reads.append(initial)
        return self.op("dve", lambda: self.nc.vector.tensor_tensor_scan(out.ap, d0.ap, d1.ap, a, op0, op1), reads, [out])

    def memset(self, out, val, eng="dve"):
        return self.op(eng, lambda: self._e(eng).memset(out.ap, val), [], [out])

    def recip(self, out, in_):
        return self.op("dve", lambda: self.nc.vector.reciprocal(out.ap, in_.ap), [in_], [out])

    def reduce(self, out, in_, op, axis=AX.X):
        return self.op("dve", lambda: self.nc.vector.tensor_reduce(out.ap, in_.ap, axis, op), [in_], [out])

import math
import numpy as np

G = 512
NT = 4
C = 64
HG = 256
NCH = HG // C
OFF_A, OFF_B, OFF_C, OFF_D, OFF_T1, OFF_MIX = 0, 128, 256, 384, 512, 770
N_MIX = 448
OFF_E, OFF_F, OFF_GD, OFF_T2 = 0, 128, 256, 384
NCOL = OFF_MIX + N_MIX
NCOLB = OFF_MIX + 2 * N_MIX
LWC = 0.6065306597126334


def vr(v, pat, **kw):
    return V(v.ap.rearrange(pat, **kw), v.bufs)


def vb(v, shape):
    return V(v.ap.broadcast_to(list(shape)), v.bufs)


def build_A(S):
    NG = S // G
    NTT = S // 128
    K = Kern()
    xT = K.dram("xT", [1024, S], F32, "ExternalInput")
    W = K.dram("W", [1024, NCOL], F32, "ExternalInput")
    mu = K.dram("mu", [1, N_MIX], F32, "ExternalInput")
    pcol = K.dram("pcol", [64, 8], F32, "ExternalInput")
    w2h = K.dram("w2h", [64, 64], F32, "ExternalInput")
    a2h = K.dram("a2h", [64, 64], F32, "ExternalInput")
    g2h = K.dram("g2h", [128, 64], F32, "ExternalInput")
    prow = K.dram("prow", [1, 194], F32, "ExternalInput")
    cst = K.dram("cst", [128, 512], F32, "ExternalInput")
    cst64 = K.dram("cst64", [64, 192], F32, "ExternalInput")
    rst = K.dram("rst", [64, HG], F32, "ExternalInput")
    rot = K.dram("rot", [64, 2, S], F32, "ExternalInput")
    retc = K.dram("retc", [128, 642], F32, "ExternalInput")
    y = K.dram("y", [S, 256], F32, "ExternalOutput")

    sb = K.sbuf
    wb = sb("wb", [128, 8, NCOLB], BF16)
    stg = [sb(f"stg{i}", [128, 1280], F32) for i in range(2)]
    mub = sb("mub", [128, N_MIX], F32)
    wtmp = sb("wtmp", [128, N_MIX], F32)
    pc = sb("pc", [64, 8], F32)
    w2b = sb("w2b", [64, 64], BF16); a2b = sb("a2b", [64, 64], BF16); g2b = sb("g2b", [128, 64], BF16)
    prb = sb("prb", [128, 194], F32)
    cs = sb("cs", [128, 512], F32)
    cs64 = sb("cs64", [64, 192], F32)
    rstm = sb("rstm", [64, HG], F32)
    rc = sb("rc", [128, 642], F32)
    causal = sb("causal", [128, 128], BF16)
    c8 = sb("c8", [1, 128], BF16)
    ident = cs[:, 0:128]; ucum = cs[:, 128:256]; ones = cs[:, 256:384]
    I64 = cs[0:64, 0:64]

    def m3(i):
        return vb(vr(cs64[:, 64 * i:64 * i + 64], "p (o t) -> p o t", o=1), [64, NCH, C])
    mUs, mLs, mUi = m3(0), m3(1), m3(2)
    I3 = vb(vr(I64, "p (o t) -> p o t", o=1), [64, NCH, C])

    xb = [sb(f"xb{i}", [128, 8, G + 1], BF16) for i in range(2)]
    rott = sb("rott", [64, 2, G], F32)

    Kc = sb("Kc", [128, S], BF16)
    Vc = sb("Vc", [128, NTT, 2, 65], BF16)
    kbufs = [Buf(f"kg{g}") for g in range(NG)]
    vbufs = [Buf(f"vg{g}") for g in range(NG)]
    ctab = sb("ctab", [128, 2, NTT], F32)
    cbufs = [Buf(f"cg{g}") for g in range(NG)]
    carry = [sb(f"carry{i}", [128, 2], F32) for i in range(2)]
    Qa = sb("Qa", [128, G], BF16)
    rqrow = [sb(f"rqrow{h}", [1, G], BF16) for h in range(2)]
    spg = sb("spg", [128, NT, 2], F32)
    spe = sb("spe", [128, NT, 2], F32)
    win = sb("win", [128, 2, NT], F32)
    inc = sb("inc", [128, 2, NT], F32)
    rq = sb("rq", [128, 2, NT], F32)
    biasg = [sb(f"biasg{h}", [128, NTT], F32) for h in range(2)]
    pts = [sb(f"pt{i}", [128, G], BF16) for i in range(3)]
    rcp = sb("rcp", [128, NT], F32)
    yfox = sb("yfox", [128, NT, 128], F32)

    P = [K.psum(f"P{i}", [128, 512], F32) for i in range(8)]

    w2f = stg[0][0:64, 0:64]; a2f = stg[0][0:64, 64:128]; g2f = stg[0][:, 128:192]
    K.dma("sp", pc, pcol)
    K.dma("sp", w2f, w2h); K.dma("sp", a2f, a2h); K.dma("sp", g2f, g2h)
    K.dma("sp", cs, cst); K.dma("sp", cs64, cst64); K.dma("sp", rstm, rst); K.dma("sp", rc, retc)
    K.dma("sp", mub, V(mu.ap.partition_broadcast(128), mu.bufs))
    K.dma("sp", prb, V(prow.ap.partition_broadcast(128), prow.bufs))
    K.copy(w2b, w2f); K.copy(a2b, a2f); K.copy(g2b, g2f)
    K.copy(causal, cs[:, 384:512])
    K.memset(c8, 8.0)
    K.ts(pc[:, 4:5], pc[:, 3:4], -1.0, ALU.mult, 1.0, ALU.add)
    Wv = vr(W, "(kt p) c -> kt p c", p=128)
    for kt in range(8):
        st = stg[kt % 2]
        K.dma("sp", st[:, 0:NCOL], Wv[kt])
        K.copy(wb[:, kt, 0:OFF_MIX], st[:, 0:OFF_MIX], eng="act")
        K.tt(wtmp, st[:, OFF_MIX:NCOL], mub, ALU.mult)
        K.copy(wb[:, kt, OFF_MIX + N_MIX:NCOLB], wtmp, eng="pool")
        K.tt(wb[:, kt, OFF_MIX:OFF_MIX + N_MIX], st[:, OFF_MIX:NCOL], wtmp, ALU.subtract)
    K.memset(Vc[:, :, :, 64:65].with_bufs(vbufs), 1.0)
    K.memset(carry[0], 0.0)
    K.memset(xb[1][:, :, G:G + 1], 0.0)

    xTv = vr(xT, "(kt p) s -> p kt s", p=128)

    def load_x(g, q):
        st = stg[q % 2]
        sv = vr(st[:, 0:1024], "p (k s) -> p k s", k=2)
        K.dma("sp", sv, xTv[:, 2 * q:2 * q + 2, g * G:(g + 1) * G])
        K.copy(xb[g % 2][:, 2 * q:2 * q + 2, 1:G + 1], sv, eng=("act" if q % 2 == 0 else "dve"))

    out_toks = []

    ST = [sb(f"ST{i}", [64, 64], F32) for i in range(2)]
    K.memset(ST[0], 0.0)
    Rst = [sb(f"Rst{i}", [64, 64], F32) for i in range(2)]
    Rb = [sb(f"Rb{i}", [64, 64], BF16) for i in range(2)]
    K.memset(Rst[0], 0.0); K.memset(Rb[0], 0.0)
    st_idx = [0]
    r_idx = [0]

    A = [sb(f"A{i}", [64, HG], F32) for i in range(28)]
    thw = sb("thw", [64, HG], BF16); adb = sb("adb", [64, HG], BF16); sgd = sb("sgd", [128, HG], BF16)
    vtok = sb("vtok", [64, 2 * NCH, 64], F32)
    st1 = sb("st1", [64, NCH], F32); st2 = sb("st2", [64, NCH], F32); rkv = sb("rkv", [64, NCH], F32)
    yrwo = [sb(f"yrwo{i}", [64, NCH, 64], F32) for i in range(2)]
    qrT = sb("qrT", [64, G], BF16); krT = sb("krT", [64, G], BF16); qdT = sb("qdT", [64, G], BF16)
    krTf = sb("krTf", [64, G], F32)
    vret = sb("vret", [128, NT, 64], BF16)
    gret = sb("gret", [128, NT, 64], F32)
    sTr = [sb(f"sTr{i}", [128, 128], BF16) for i in range(2)]
    kdt = [sb(f"kdt{i}", [128, 64], BF16) for i in range(2)]
    yr = sb("yr", [128, NT, 64], F32); yrc = sb("yrc", [128, NT, 64], F32); yrs = sb("yrs", [128, NT, 64], F32)
    rs1 = sb("rs1", [128, NT], F32); rs2 = sb("rs2", [128, NT], F32)
    yreto = sb("yreto", [128, NT, 64], F32)

    def groupnorm(yv, ycv, ysqv, s1, s2, eps, Pn, n):
        K.reduce(s1, yv, ALU.add)
        K.ts(s1, s1, -1.0 / 64, ALU.mult)
        K.tt(ycv, yv, vb(vr(s1, "p (n o) -> p n o", o=1), [Pn, n, 64]), ALU.add)
        K.tt(ysqv, ycv, ycv, ALU.mult)
        K.reduce(s2, ysqv, ALU.add)
        K.ts(s2, s2, 1.0 / 64, ALU.mult, float(eps), ALU.add)
        K.act(s2, s2, AF.Ln)
        K.act(s2, s2, AF.Exp, scale=-0.5)
        K.tt(ycv, ycv, vb(vr(s2, "p (n o) -> p n o", o=1), [Pn, n, 64]), ALU.mult)

    def c3(v):
        return vr(v, "p (c t) -> p c t", c=NCH)

    def ch(v, c):
        return v[:, c * C:(c + 1) * C]

    for q in range(4):
        load_x(0, q)

    for g in range(NG):
        cur, prv = g % 2, (g + 1) % 2
        t0 = g * G
        K.copy(xb[cur][:, :, 0:1], xb[prv][:, :, G:G + 1], eng="pool")
        X = xb[cur]
        K.dma("sp", rott, rot[:, :, t0:t0 + G])

        def proj_fm(pout, off, mixed, c0=0, n=G):
            for kt in range(8):
                if not mixed:
                    K.mm(pout, wb[:, kt, off:off + 128], X[:, kt, 1 + c0:1 + c0 + n], start=(kt == 0), stop=(kt == 7))
                else:
                    K.mm(pout, wb[:, kt, OFF_MIX + off:OFF_MIX + off + 128], X[:, kt, 1 + c0:1 + c0 + n], start=(kt == 0), stop=False)
                    K.mm(pout, wb[:, kt, OFF_MIX + N_MIX + off:OFF_MIX + N_MIX + off + 128], X[:, kt, c0:c0 + n], start=False, stop=(kt == 7))

        proj_fm(P[0], OFF_A, False)
        K.copy(Qa, P[0], eng="act")
        proj_fm(P[1], OFF_B, False)
        K.copy(Kc[:, t0:t0 + G].with_bufs([kbufs[g]]), P[1], eng="dve")
        for tt_ in range(NT):
            pb = tt_ % 2
            tsl = slice(1 + tt_ * 128, 1 + tt_ * 128 + 128)
            tsl_prev = slice(tt_ * 128, tt_ * 128 + 128)
            for kt in range(8):
                K.mm(P[pb][:, 0:258], X[:, kt, tsl], wb[:, kt, OFF_T1:OFF_T1 + 258], start=(kt == 0), stop=(kt == 7))
            for kt in range(8):
                K.mm(P[pb][:, 258:322], X[:, kt, tsl], wb[:, kt, OFF_MIX + OFF_T2:OFF_MIX + OFF_T2 + 64], start=(kt == 0), stop=False)
                K.mm(P[pb][:, 258:322], X[:, kt, tsl_prev], wb[:, kt, OFF_MIX + N_MIX + OFF_T2:OFF_MIX + N_MIX + OFF_T2 + 64], start=False, stop=(kt == 7))
            tile = g * NT + tt_
            K.copy(Vc[:, tile, :, 0:64].with_bufs([vbufs[g]]), vr(P[pb][:, 0:128], "p (h d) -> p h d", h=2), eng="act")
            K.tt(spg[:, tt_, :], P[pb][:, 128:130], prb[:, 0:2], ALU.add)
            K.copy(vret[:, tt_, :], P[pb][:, 130:194], eng="dve")
            K.act(gret[:, tt_, :], P[pb][:, 194:258], AF.Silu)
            for hh in range(2):
                K.copy(vtok[:, tt_ * 2 + hh, :], P[pb][64 * hh:64 * hh + 64, 258:322], eng="dve")
        K.act(spe, spg, AF.Exp, scale=-1.0)
        K.ts(spe, spe, 1.0, ALU.add)
        K.act(spe, spe, AF.Ln)
        spv = vr(spe, "p t h -> p (t h)")
        K.mm(P[0][:, 0:8], ucum, spv)
        K.mm(P[0][:, 8:16], ones, spv)
        pv4 = vr(P[0][:, 0:16], "p (a t h) -> p a h t", a=2, h=2)
        K.copy(win, pv4[:, 0], eng="dve")
        K.copy(rq, pv4[:, 1], eng="dve")
        for h in range(2):
            K.scan(inc[:, h, :], ones[:, 0:NT], rq[:, h, :], carry[cur][:, h:h + 1], ALU.mult, ALU.add)
        K.tt(win, win, inc, ALU.add)
        K.tt(ctab[:, :, g * NT:(g + 1) * NT].with_bufs([cbufs[g]]), win, rq, ALU.subtract)
        K.copy(carry[prv], inc[:, :, NT - 1], eng="dve")
        K.tt(rq, vb(inc[:, :, NT - 1:NT], [128, 2, NT]), inc, ALU.subtract)
        nk = (g + 1) * NT
        for h in range(2):
            K.copy(vr(rqrow[h], "p (j t) -> p j t", j=NT),
                   vb(vr(rq[0:1, h, :], "p (j o) -> p j o", o=1), [1, NT, 128]), eng="dve")
            K.ts(biasg[h][:, 0:nk], ctab[:, h, 0:nk].with_bufs(cbufs[0:g + 1]), inc[:, h, NT - 1:NT], ALU.subtract)
        if g + 1 < NG:
            for q in range(4):
                load_x(g + 1, q)
        K.defer = []
        for (off, pbk, dst, dstf) in ((OFF_C, 0, qrT, None), (OFF_D, 1, krT, krTf)):
            proj_fm(P[pbk], off, False)
            for hf in range(2):
                hsl = slice(hf * HG, (hf + 1) * HG)
                K.tt(A[0], P[pbk][0:64, hsl], rott[:, 0, hsl], ALU.mult)
                K.tt(A[1], P[pbk][64:128, hsl], rott[:, 1, hsl], ALU.mult)
                if dstf is None:
                    K.tt(dst[:, hsl], A[0], A[1], ALU.add)
                else:
                    K.tt(dstf[:, hsl], A[0], A[1], ALU.add)
                    K.copy(dst[:, hsl], dstf[:, hsl], eng="act")
        K.tt(qdT, qrT, rc[0:64, 129:641], ALU.mult)
        Yr = vr(P[6][:, 0:NT * 64], "p (j e) -> p j e", j=NT)
        for n in range(NT):
            csl = slice(n * 128, (n + 1) * 128)
            K.mm(P[7][:, 0:128], krT[:, csl], qrT[:, csl])
            sT = sTr[n % 2]
            K.tt(sT, P[7][:, 0:128], rc[:, 0:128], ALU.mult)
            K.transpose(P[7][:, 256:320], krTf[:, csl], I64)
            kd = kdt[n % 2]
            K.ts(kd, P[7][:, 256:320], rc[:, 128:129], ALU.mult)
            ri = r_idx[0]
            K.mm(Yr[:, n, :], sT, vret[:, n, :], start=True, stop=False)
            K.mm(Yr[:, n, :], qdT[:, csl], Rb[ri], start=False, stop=True)
            K.mm(P[7][0:64, 384:448], kd, vret[:, n, :])
            K.stt(Rst[1 - ri], Rst[ri], rc[0:64, 641:642], P[7][0:64, 384:448], ALU.mult, ALU.add)
            K.copy(Rb[1 - ri], Rst[1 - ri], eng="act")
            r_idx[0] = 1 - ri
        K.copy(yr, Yr, eng="act")
        groupnorm(yr, yrc, yrs, rs1, rs2, 1e-6, 128, NT)
        K.tt(yrc, yrc, vb(vr(prb[:, 130:194], "p (o e) -> p o e", o=1), [128, NT, 64]), ALU.mult)
        K.tt(yreto, yrc, gret, ALU.mult)
        yo = V(y.ap[t0:t0 + G, 192:256].rearrange("(j p) c -> p j c", p=128), [Buf()])
        K.dma("pool", yo, yreto, owner=yreto.bufs[0], cb=out_toks.append)

        for hf in range(2):
            c0 = hf * HG
            (rT, kT, sig, aT, kk, sq, rn, bT, ktl, tmpa, csum, epos, eneg, eprev,
             KpT, BpT, KtT, RpT, BppT, KtppT, Dg, rkr, Pm, PTm, AbrT, AkrT, gtok, spare) = A
            QtT, nLkV, Um, Wm, Gm, HT, yrw = rT, kT, sig, aT, kk, sq, rn
            Kptok, Bpptok, Ktpptok, yc_, ysq, LkTm, MT = bT, ktl, tmpa, csum, epos, eneg, eprev
            p0 = P[0][0:64, 0:HG]; p1 = P[1][0:64, 0:HG]
            proj_fm(P[0][:, 0:HG], OFF_E, True, c0, HG)
            K.copy(rT, P[0][0:64, 0:HG], eng="act")
            K.copy(kT, P[0][64:128, 0:HG], eng="dve")
            proj_fm(P[1][:, 0:HG], OFF_F, True, c0, HG)
            K.act(thw, P[1][0:64, 0:HG], AF.Tanh)
            K.copy(adb, P[1][64:128, 0:HG], eng="dve")
            proj_fm(P[0][:, 0:HG], OFF_GD, True, c0, HG)
            K.act(sgd, P[0][:, 0:HG], AF.Sigmoid)
            K.mm(p1, w2b, thw)
            K.act(sig, p1, AF.Sigmoid, bias=pc[:, 0:1])
            K.mm(p0, a2b, adb)
            K.act(aT, p0, AF.Sigmoid, bias=pc[:, 1:2])
            for t2 in range(2):
                K.mm(P[1][:, 256:320], sgd[:, t2 * 128:(t2 + 1) * 128], g2b)
                for hh in range(2):
                    cch = t2 * 2 + hh
                    K.copy(ch(gtok, cch), P[1][64 * hh:64 * hh + 64, 256:320], eng="act")
            K.ts(kk, kT, pc[:, 2:3], ALU.mult)
            K.tt(sq, kk, kk, ALU.mult)
            K.mm(p0, ones[0:64, 0:64], sq)
            K.act(rn, p0, AF.Sqrt)
            K.ts(rn, rn, 1e-12, ALU.max)
            K.recip(rn, rn)
            K.tt(kk, kk, rn, ALU.mult)
            K.tt(bT, kk, aT, ALU.mult)
            K.ts(tmpa, aT, pc[:, 3:4], ALU.mult, pc[:, 4:5], ALU.add)
            K.tt(ktl, kT, tmpa, ALU.mult)
            K.tt(rkr, rT, ktl, ALU.mult)
            K.ts(rkr, rkr, pc[:, 5:6], ALU.mult)
            K.scan(csum, rstm, sig, 0.0, ALU.mult, ALU.add)
            K.act(epos, csum, AF.Exp, scale=-LWC)
            K.act(eneg, csum, AF.Exp, scale=LWC)
            K.tt(tmpa, csum, sig, ALU.subtract)
            K.act(eprev, tmpa, AF.Exp, scale=-LWC)
            K.tt(KpT, kk, eprev, ALU.mult)
            K.tt(BpT, bT, eneg, ALU.mult)
            K.tt(KtT, ktl, eneg, ALU.mult)
            K.tt(RpT, rT, epos, ALU.mult)
            eCb = vb(c3(epos)[:, :, C - 1:C], [64, NCH, C])
            K.tt(c3(BppT), c3(BpT), eCb, ALU.mult)
            K.tt(c3(KtppT), c3(KtT), eCb, ALU.mult)
            K.tt(c3(Dg), I3, eCb, ALU.mult)

            def pcm(pout, lhs, rhs):
                for c in range(NCH):
                    K.mm(ch(pout, c), ch(lhs, c), ch(rhs, c))
            pcm(p0, BpT, KpT)
            K.stt(c3(PTm), c3(p0), -1.0, mUs, ALU.mult, ALU.mult)
            pcm(p1, KpT, BpT)
            K.stt(c3(Pm), c3(p1), -1.0, mLs, ALU.mult, ALU.mult)
            pcm(p0, KtT, KpT)
            K.tt(c3(LkTm), c3(p0), mUs, ALU.mult)
            pcm(p1, BpT, RpT)
            K.tt(c3(AbrT), c3(p1), mUi, ALU.mult)
            pcm(p0, KtT, RpT)
            K.tt(c3(AkrT), c3(p0), mUi, ALU.mult)
            K.tt(c3(MT), c3(PTm), I3, ALU.add)
            for lvl in range(1, 6):
                pcm(p0, PTm, Pm)
                pcm(p1, Pm, PTm)
                K.copy(Pm, p0, eng="act")
                K.copy(PTm, p1, eng="dve")
                pcm(p0, Pm, MT)
                K.tt(MT, MT, p0, ALU.add)
            for src, dst, pp in ((KpT, Kptok, p0), (BppT, Bpptok, p1), (KtppT, Ktpptok, p0)):
                for c in range(NCH):
                    K.transpose(ch(pp, c), ch(src, c), I64)
                K.copy(dst, pp, eng=("act" if pp is p0 else "dve"))
            vh = vr(vtok[:, hf * NCH:(hf + 1) * NCH, :], "p c e -> p (c e)")
            pcm(p1, LkTm, vh)
            K.ts(nLkV, p1, -1.0, ALU.mult)
            pcm(p0, MT, nLkV)
            K.copy(Um, p0, eng="act")
            pcm(p1, MT, Kptok)
            K.copy(Wm, p1, eng="dve")
            pcm(p0, Wm, Bpptok)
            K.tt(Gm, Dg, p0, ALU.subtract)
            for c in range(NCH):
                K.mm(ch(p1, c), ch(Bpptok, c), ch(Um, c), start=True, stop=False)
                K.mm(ch(p1, c), ch(Ktpptok, c), ch(vh, c), start=False, stop=True)
            K.copy(HT, p1, eng="act")
            pcm(p0, Wm, AbrT)
            K.tt(QtT, RpT, p0, ALU.subtract)
            for c in range(NCH):
                K.mm(P[1][0:64, 448 + c:449 + c], ch(rkr, c), ones[0:64, 0:1])
            K.copy(rkv, P[1][0:64, 448:448 + NCH], eng="dve")
            Yp = P[6][0:64, 256:256 + HG]
            for c in range(NCH):
                si = st_idx[0]
                K.mm(ch(Yp, c), ch(QtT, c), ST[si], start=True, stop=False)
                K.mm(ch(Yp, c), ch(AbrT, c), ch(Um, c), start=False, stop=False)
                K.mm(ch(Yp, c), ch(AkrT, c), ch(vh, c), start=False, stop=True)
                K.mm(P[7][0:64, 448:512], ch(Gm, c), ST[si])
                K.tt(ST[1 - si], P[7][0:64, 448:512], ch(HT, c), ALU.add)
                st_idx[0] = 1 - si
            K.copy(yrw, Yp, eng="act")
            groupnorm(c3(yrw), c3(yc_), c3(ysq), st1, st2, 64e-5, 64, NCH)
            K.tt(c3(yc_), c3(yc_), vb(vr(prb[0:64, 2:66], "p (o e) -> p o e", o=1), [64, NCH, 64]), ALU.mult)
            K.tt(c3(yc_), c3(yc_), vb(vr(prb[0:64, 66:130], "p (o e) -> p o e", o=1), [64, NCH, 64]), ALU.add)
            K.tt(c3(ysq), c3(vh), vb(vr(rkv, "p (c o) -> p c o", o=1), [64, NCH, 64]), ALU.mult)
            K.tt(yc_, yc_, ysq, ALU.add)
            yo_t = yrwo[hf]
            K.tt(vr(yo_t, "p c e -> p (c e)"), yc_, gtok, ALU.mult)
            yo = V(y.ap[t0 + c0:t0 + c0 + HG, 128:192].rearrange("(c p) e -> p c e", p=C), [Buf()])
            K.dma("pool", yo, yo_t, owner=yo_t.bufs[0], cb=out_toks.append)

        deferred = K.defer
        K.defer = None
        pti = 0
        n_iter = 2 * nk
        per = max(1, -(-len(deferred) // n_iter))
        dpos = 0
        for h in range(2):
            hs = slice(64 * h, 64 * h + 64)
            PV = vr(P[4 + h][:, 0:NT * 65], "p (j e) -> p j e", j=NT)
            def emit_st(kt):
                j = kt - g * NT
                q0 = 128 * max(j, 0)
                ncol = G - q0
                pb = 2 + (kt % 2)
                kg = kt // NT
                K.mm(P[pb][:, 0:ncol], Kc[hs, kt * 128:(kt + 1) * 128].with_bufs([kbufs[kg]]), Qa[hs, q0:G], start=True, stop=False)
                K.mm(P[pb][:, 0:ncol], c8, rqrow[h][:, q0:G], start=False, stop=True)
            emit_st(0)
            for kt in range(nk):
                if kt + 1 < nk:
                    emit_st(kt + 1)
                j = kt - g * NT
                q0 = 128 * max(j, 0)
                ncol = G - q0
                pb = 2 + (kt % 2)
                kg = kt // NT
                pt = pts[pti % 3]; pti += 1
                K.act(pt[:, 0:ncol], P[pb][:, 0:ncol], AF.Exp, bias=biasg[h][:, kt:kt + 1], scale=0.125)
                if j >= 0:
                    K.tt(pt[:, 0:128], pt[:, 0:128], causal, ALU.mult, eng="pool")
                for jj in range(max(j, 0), NT):
                    cc0 = (jj - max(j, 0)) * 128
                    K.mm(PV[:, jj, :], pt[:, cc0:cc0 + 128], Vc[:, kt, h, :].with_bufs([vbufs[kg]]),
                         start=(kt == 0 and jj == 0), stop=(kt == g * NT + jj), skip=True)
                K.replay(deferred[dpos:dpos + per]); dpos += per
            K.recip(rcp, PV[:, :, 64])
            K.tt(yfox[:, :, 64 * h:64 * h + 64], PV[:, :, 0:64], vb(vr(rcp, "p (j o) -> p j o", o=1), [128, NT, 64]), ALU.mult)
        yo = V(y.ap[t0:t0 + G, 0:128].rearrange("(j p) c -> p j c", p=128), [Buf()])
        out_toks.append(K.dma("pool", yo, yfox, owner=yfox.bufs[0]))
        K.replay(deferred[dpos:])


    K.finish(out_toks)
    return K


def host_consts(S, head):
    ident = np.eye(128, dtype=np.float32)
    ucum = np.triu(np.ones((128, 128), np.float32))
    ones = np.ones((128, 128), np.float32)
    causal = np.triu(np.ones((128, 128), np.float32))
    cst = np.concatenate([ident, ucum, ones, causal], 1)
    mUs = np.triu(np.ones((64, 64), np.float32), 1)
    mLs = np.tril(np.ones((64, 64), np.float32), -1)
    mUi = np.triu(np.ones((64, 64), np.float32), 0)
    cst64 = np.concatenate([mUs, mLs, mUi], 1)
    rst = np.ones((64, HG), np.float32); rst[:, ::64] = 0.0
    half = 32
    inv = 10000.0 ** (-np.arange(half, dtype=np.float64) / half)
    inv = inv.astype(np.float32).astype(np.float64)
    pos = np.arange(S, dtype=np.float64)
    ang = (pos[None, :].astype(np.float32) * inv[:, None].astype(np.float32)).astype(np.float64)
    cos, sin = np.cos(ang), np.sin(ang)
    CC = np.concatenate([cos, cos], 0)
    SS = np.concatenate([-sin, sin], 0)
    rot = np.ascontiguousarray(np.stack([CC, SS], 1).astype(np.float32))
    lg = np.log1p(-np.exp2(-5.0 - head))
    idx = np.arange(128, dtype=np.float64)
    diff = idx[None, :] - idx[:, None]
    dmT = np.where(diff >= 0, np.exp(np.maximum(diff, 0) * lg), 0.0) * 0.125
    kdec = np.exp((127.0 - idx) * lg) * 0.125
    qdec = np.exp((idx + 1.0) * lg)
    retc = np.zeros((128, 642), np.float32)
    retc[:, 0:128] = dmT
    retc[:, 128] = kdec
    retc[:, 129:641] = np.tile(qdec, 4)[None, :]
    retc[:, 641] = np.exp(128 * lg)
    return dict(cst=cst, cst64=cst64, rst=rst, rot=rot, retc=retc)


IN_SPL = (512, 512, 512, 8, 256, 256, 256, 64, 64, 128, 256, 256, 256, 256)
IN_OFF = np.concatenate([[0], np.cumsum(IN_SPL)])


def host_layer_inputs(inp, l, j):
    w_in = np.asarray(inp["w_in"][l])
    o = IN_OFF
    fq, fk, fv, ff, rr, rk, rv, rwd, rad, rgd, tq, tk, tv, tg = [w_in[:, o[i]:o[i + 1]] for i in range(14)]
    h0, h1 = 2 * j, 2 * j + 1
    sl = lambda w, h: w[:, 64 * h:64 * h + 64]
    sw = lambda w: np.concatenate([w[:, 32:64], w[:, 0:32]], 1)
    cols = [sl(fq, h0), sl(fq, h1), sl(fk, h0), sl(fk, h1),
            sl(tq, j), sw(sl(tq, j)), sl(tk, j), sw(sl(tk, j)),
            sl(fv, h0), sl(fv, h1), ff[:, h0:h0 + 2], sl(tv, j), sl(tg, j),
            sl(rr, j), sl(rk, j), rwd, rad, rgd, sl(rv, j)]
    W = np.ascontiguousarray(np.concatenate(cols, 1), dtype=np.float32)
    assert W.shape[1] == NCOL
    mr = np.asarray(inp["rwkv_mu_rkv"][l]); ml = np.asarray(inp["rwkv_mu_lora"][l])
    c64 = slice(64 * j, 64 * j + 64)
    mu = np.concatenate([mr[0, c64], mr[1, c64], ml, mr[2, c64]])[None, :].astype(np.float32)
    pcol = np.zeros((64, 8), np.float32)
    pcol[:, 0] = np.asarray(inp["rwkv_w0"][l])[c64]
    pcol[:, 1] = np.asarray(inp["rwkv_a0"][l])[c64]
    pcol[:, 2] = np.asarray(inp["rwkv_k_k"][l])[c64]
    pcol[:, 3] = np.asarray(inp["rwkv_k_a"][l])[c64]
    pcol[:, 5] = np.asarray(inp["rwkv_r_k"][l])[j]
    prow = np.concatenate([np.asarray(inp["fox_fgate_bias"][l])[h0:h0 + 2],
                           np.asarray(inp["rwkv_ln_g"][l])[c64], np.asarray(inp["rwkv_ln_b"][l])[c64],
                           np.asarray(inp["ret_gn_g"][l])[c64]])[None, :].astype(np.float32)
    return dict(W=W, mu=mu, pcol=pcol, prow=prow,
                w2h=np.ascontiguousarray(np.asarray(inp["rwkv_w2"][l])[:, c64]),
                a2h=np.ascontiguousarray(np.asarray(inp["rwkv_a2"][l])[:, c64]),
                g2h=np.ascontiguousarray(np.asarray(inp["rwkv_g2"][l])[:, c64]))

import numpy as np

ALPHA = (2.0 * 4) ** 0.25
LN_EPS = 1e-5
NE = 32


def build_B(NTOK, GN):
    NGR = NTOK // GN
    NTL = GN // 128
    HB = min(512, GN)
    NHB = GN // HB
    K = Kern()
    ycT = K.dram("ycT", [1024, NTOK], F32, "ExternalInput")
    xin = K.dram("xin", [NTOK, 1024], F32, "ExternalInput")
    wout = K.dram("wout", [1024, 1024], F32, "ExternalInput")
    lnp = K.dram("lnp", [1, 4096], F32, "ExternalInput")
    wr = K.dram("wr", [1024, 36], F32, "ExternalInput")
    br = K.dram("br", [1, 36], F32, "ExternalInput")
    w1 = K.dram("w1", [NE, 1024, 512], F32, "ExternalInput")
    w3 = K.dram("w3", [NE, 1024, 512], F32, "ExternalInput")
    w2 = K.dram("w2", [NE, 512, 1024], F32, "ExternalInput")
    idn = K.dram("idn", [128, 128], F32, "ExternalInput")
    out = K.dram("out", [NTOK, 1024], F32, "ExternalOutput")

    sb = K.sbuf
    ident = sb("ident", [128, 128], F32)
    lnb = sb("lnb", [128, 4096], F32)
    wrb = sb("wrb", [128, 8, 36], F32)
    brb = sb("brb", [128, 36], F32)
    woutb = sb("woutb", [128, 8, 1024], BF16)
    stg = [sb(f"stg{i}", [128, 2048], F32) for i in range(2)]
    w1b = [sb(f"w1b{i}", [128, 8, 512], BF16) for i in range(2)]
    w3b = [sb(f"w3b{i}", [128, 8, 512], BF16) for i in range(2)]
    w2b = [sb(f"w2b{i}", [128, 4, 1024], BF16) for i in range(2)]
    x1 = sb("x1", [128, NTL, 1024], F32)
    yacc = sb("yacc", [128, NTL, 1024], F32)
    x1Tb = sb("x1Tb", [128, 8, GN], BF16)
    x1Tf = sb("x1Tf", [128, 8, 128], F32)
    ycb = sb("ycb", [128, 8, 128], BF16)
    hb = sb("hb", [128, 1024], F32)
    hc = sb("hc", [128, 1024], F32)
    gates = sb("gates", [128, NTL, NE], F32)
    lg = sb("lg", [128, 36], F32)
    sm = [sb(f"sm{i}", [128, 8], F32) for i in range(10)]
    mg = sb("mg", [128, 4], F32)
    esel = sb("esel", [128, 8], F32); e2 = sb("e2", [128, 8], F32)
    tmp32 = sb("tmp32", [128, 32], F32)
    ge = sb("ge", [128, 8], F32)
    st = [sb(f"lnst{i}", [128, 1], F32) for i in range(4)]
    sil = [sb(f"sil{i}", [128, HB], BF16) for i in range(2)]
    hg = [sb(f"hg{i}", [128, NHB, HB], BF16) for i in range(4)]

    P = [K.psum(f"P{i}", [128, 512], F32) for i in range(8)]

    K.dma("sp", ident, idn)
    K.dma("sp", lnb, V(lnp.ap.partition_broadcast(128), lnp.bufs))
    K.dma("sp", brb, V(br.ap.partition_broadcast(128), br.bufs))
    K.dma("sp", wrb, vr(wr, "(kt p) c -> p kt c", p=128))
    wov = vr(wout, "(kt p) c -> p kt c", p=128)
    for q in range(4):
        sv = vr(stg[q % 2], "p (k c) -> p k c", k=2)
        K.dma("sp", sv, wov[:, 2 * q:2 * q + 2, :])
        K.copy(woutb[:, 2 * q:2 * q + 2, :], sv, eng=("act" if q % 2 == 0 else "dve"))

    def layernorm(dst, src, goff, boff):
        K.reduce(st[0], src, ALU.add)
        K.ts(st[0], st[0], -1.0 / 1024, ALU.mult)
        K.ts(hc, src, st[0], ALU.add)
        K.act(dst, hc, AF.Square, accum_out=st[1])
        K.ts(st[1], st[1], 1.0 / 1024, ALU.mult, LN_EPS, ALU.add)
        K.act(st[1], st[1], AF.Ln)
        K.act(st[1], st[1], AF.Exp, scale=-0.5)
        K.ts(hc, hc, st[1], ALU.mult)
        K.tt(hc, hc, lnb[:, goff:goff + 1024], ALU.mult)
        K.tt(dst, hc, lnb[:, boff:boff + 1024], ALU.add)

    ycTv = vr(ycT, "(kt p) s -> p kt s", p=128)
    xinv = vr(xin, "(n p) d -> n p d", p=128)
    outv = vr(out, "(n p) d -> n p d", p=128)
    w1v = vr(w1, "e (kt p) c -> e p kt c", p=128)
    w3v = vr(w3, "e (kt p) c -> e p kt c", p=128)
    w2v = vr(w2, "e (kt p) c -> e p kt c", p=128)
    out_toks = []
    wcount = [0]

    for gr in range(NGR):
        for tl in range(NTL):
            tile = gr * NTL + tl
            tok0 = tile * 128
            sv = vr(stg[0][:, 0:1024], "p (k c) -> p k c", k=8)
            K.dma("sp", sv, ycTv[:, :, tok0:tok0 + 128])
            K.copy(ycb, sv, eng="act")
            K.dma("sp", stg[1][:, 0:1024], xinv[tile])
            for half in range(2):
                for kt in range(8):
                    K.mm(P[half], ycb[:, kt, :], woutb[:, kt, 512 * half:512 * half + 512], start=(kt == 0), stop=(kt == 7))
            for half in range(2):
                K.stt(hb[:, 512 * half:512 * half + 512], stg[1][:, 512 * half:512 * half + 512], ALPHA, P[half], ALU.mult, ALU.add)
            layernorm(x1[:, tl, :], hb, 0, 1024)
            for half in range(2):
                for k4 in range(4):
                    kt = half * 4 + k4
                    K.transpose(P[2 + half][:, k4 * 128:(k4 + 1) * 128], x1[:, tl, kt * 128:(kt + 1) * 128], ident)
                K.copy(vr(x1Tf[:, 4 * half:4 * half + 4, :], "p k t -> p (k t)"), P[2 + half], eng=("act" if half == 0 else "dve"))
            K.copy(x1Tb[:, :, tl * 128:(tl + 1) * 128], x1Tf, eng="pool")
            for kt in range(8):
                K.mm(P[4][:, 0:36], x1Tf[:, kt, :], wrb[:, kt, :], start=(kt == 0), stop=(kt == 7))
            K.tt(lg, P[4][:, 0:36], brb, ALU.add)
            gl = lg[:, 0:4]
            gmax, gsum, m1, m2, wa, wb_, gg = sm[0][:, 0:1], sm[1][:, 0:1], sm[2][:, 0:1], sm[3][:, 0:1], sm[4][:, 0:1], sm[5][:, 0:1], sm[6][:, 0:1]
            K.reduce(gmax, gl, ALU.max)
            K.ts(mg, gl, gmax, ALU.is_equal)
            K.ts(sm[7][:, 0:4], gl, gmax, ALU.subtract)
            K.act(sm[7][:, 0:4], sm[7][:, 0:4], AF.Exp, accum_out=gsum)
            K.recip(gg, gsum)
            el = vr(lg[:, 4:36], "p (g e) -> p g e", g=4)
            K.tt(vr(tmp32, "p (g e) -> p g e", g=4), el, vb(vr(mg, "p (g o) -> p g o", o=1), [128, 4, 8]), ALU.mult)
            K.reduce(esel, vr(tmp32, "p (g e) -> p e g", g=4), ALU.add)
            K.reduce(m1, esel, ALU.max)
            K.ts(sm[8], esel, m1, ALU.is_equal)
            K.stt(e2, sm[8], -1e30, esel, ALU.mult, ALU.add)
            K.reduce(m2, e2, ALU.max)
            K.ts(sm[9], e2, m2, ALU.is_equal)
            K.tt(wa, m2, m1, ALU.subtract)
            K.act(wa, wa, AF.Exp)
            K.ts(wa, wa, 1.0, ALU.add)
            K.recip(wa, wa)
            K.ts(wb_, wa, -1.0, ALU.mult, 1.0, ALU.add)
            K.tt(wa, wa, gg, ALU.mult)
            K.tt(wb_, wb_, gg, ALU.mult)
            K.ts(ge, sm[8], wa, ALU.mult)
            K.stt(ge, sm[9], wb_, ge, ALU.mult, ALU.add)
            K.tt(vr(gates[:, tl, :], "p (g e) -> p g e", g=4), vb(vr(mg, "p (g o) -> p g o", o=1), [128, 4, 8]),
                 vb(vr(ge, "p (o e) -> p o e", o=1), [128, 4, 8]), ALU.mult)
        for e in range(NE):
            wi = wcount[0] % 2; wcount[0] += 1
            for (src, dstb, k4n, eng) in ((w1v, w1b, 8, "act"), (w3v, w3b, 8, "dve")):
                for hq in range(2):
                    s_ = stg[hq]
                    sv = vr(s_[:, 0:2048], "p (k c) -> p k c", k=4)
                    K.dma("sp", sv, src[e][:, 4 * hq:4 * hq + 4, :])
                    K.copy(dstb[wi][:, 4 * hq:4 * hq + 4, :], sv, eng=(eng if hq == 0 else "pool"))
            for hq in range(2):
                sv = vr(stg[hq][:, 0:2048], "p (k c) -> p k c", k=2)
                K.dma("sp", sv, w2v[e][:, 2 * hq:2 * hq + 2, :])
                K.copy(w2b[wi][:, 2 * hq:2 * hq + 2, :], sv, eng=("act" if hq == 0 else "dve"))
            for hbk in range(NHB):
                cs_ = slice(hbk * HB, (hbk + 1) * HB)
                for nt in range(4):
                    pa, pb = P[nt % 2], P[2 + nt % 2]
                    for kt in range(8):
                        K.mm(pa[:, 0:HB], w1b[wi][:, kt, nt * 128:(nt + 1) * 128], x1Tb[:, kt, cs_], start=(kt == 0), stop=(kt == 7))
                    for kt in range(8):
                        K.mm(pb[:, 0:HB], w3b[wi][:, kt, nt * 128:(nt + 1) * 128], x1Tb[:, kt, cs_], start=(kt == 0), stop=(kt == 7))
                    s2 = sil[nt % 2]
                    K.act(s2, pa[:, 0:HB], AF.Silu)
                    K.tt(hg[nt][:, hbk, :], s2, pb[:, 0:HB], ALU.mult)
            for tl in range(NTL):
                hbk, off = (tl * 128) // HB, (tl * 128) % HB
                for half in range(2):
                    py = P[4 + 2 * (tl % 2) + half]
                    for kt in range(4):
                        K.mm(py, hg[kt][:, hbk, off:off + 128], w2b[wi][:, kt, 512 * half:512 * half + 512], start=(kt == 0), stop=(kt == 3))
                    ysl = yacc[:, tl, 512 * half:512 * half + 512]
                    if e == 0:
                        K.ts(ysl, py, gates[:, tl, e:e + 1], ALU.mult)
                    else:
                        K.stt(ysl, py, gates[:, tl, e:e + 1], ysl, ALU.mult, ALU.add)
        for tl in range(NTL):
            tile = gr * NTL + tl
            K.stt(hb, x1[:, tl, :], ALPHA, yacc[:, tl, :], ALU.mult, ALU.add)
            layernorm(yacc[:, tl, :], hb, 2048, 3072)
            yo = V(outv.ap[tile], [Buf()])
            out_toks.append(K.dma("pool", yo, yacc[:, tl, :], owner=yacc.bufs[0]))
    K.finish(out_toks)
    return K


_CACHE = {}


def _get_A(S):
    if ("A", S) not in _CACHE:
        _CACHE[("A", S)] = build_A(S)
    return _CACHE[("A", S)]


def _get_B(NTOK, GN):
    if ("B", NTOK, GN) not in _CACHE:
        _CACHE[("B", NTOK, GN)] = build_B(NTOK, GN)
    return _CACHE[("B", NTOK, GN)]


def host_B(inp, l):
    return dict(wout=np.ascontiguousarray(inp["w_out"][l]),
                lnp=np.concatenate([inp["ln1_g"][l], inp["ln1_b"][l], inp["ln2_g"][l], inp["ln2_b"][l]])[None, :].astype(np.float32),
                wr=np.ascontiguousarray(np.concatenate([inp["moe_w_group"][l], inp["moe_w_expert"][l]], 1)),
                br=np.concatenate([inp["moe_b_group"][l], inp["moe_b_expert"][l]])[None, :].astype(np.float32),
                w1=np.ascontiguousarray(inp["moe_w1"][l]), w3=np.ascontiguousarray(inp["moe_w3"][l]),
                w2=np.ascontiguousarray(inp["moe_w2"][l]),
                idn=np.eye(128, dtype=np.float32))


def kernel(**inputs):
    inp = {k: np.asarray(v) for k, v in inputs.items()}
    x = np.ascontiguousarray(inp["x"], dtype=np.float32)
    B_, S_, D = x.shape
    NC = 8
    T = B_ * S_
    NTOK = T // NC
    depth = inp["w_in"].shape[0]
    consts = [host_consts(S_, j) for j in range(4)]
    for l in range(depth):
        KA = _get_A(S_)
        xTs = [np.ascontiguousarray(x[b].T) for b in range(B_)]
        in_maps = []
        for c in range(NC):
            b, j = c // 4, c % 4
            d = dict(xT=xTs[b])
            d.update(consts[j])
            d.update(host_layer_inputs(inp, l, j))
            in_maps.append(d)
        res = run_bass_kernel_spmd(KA.nc, in_maps, core_ids=list(range(NC)), trace=True)
        ycat = np.empty((B_, S_, D), np.float32)
        for c in range(NC):
            b, j = c // 4, c % 4
            yk = res.results[c]["y"]
            ycat[b, :, 128 * j:128 * j + 128] = yk[:, 0:128]
            ycat[b, :, 512 + 64 * j:512 + 64 * j + 64] = yk[:, 128:192]
            ycat[b, :, 768 + 64 * j:768 + 64 * j + 64] = yk[:, 192:256]
        KB = _get_B(NTOK, 1024)
        hb = host_B(inp, l)
        yf = ycat.reshape(T, D)
        xf = x.reshape(T, D)
        in_maps = []
        for c in range(NC):
            sl = slice(c * NTOK, (c + 1) * NTOK)
            d = dict(ycT=np.ascontiguousarray(yf[sl].T), xin=np.ascontiguousarray(xf[sl]))
            d.update(hb)
            in_maps.append(d)
        res = run_bass_kernel_spmd(KB.nc, in_maps, core_ids=list(range(NC)), trace=True)
        x = np.concatenate([res.results[c]["out"] for c in range(NC)], 0).reshape(B_, S_, D)
    return x.astype(np.float32)
```

```python
import bisect
from contextlib import ExitStack
import numpy as np
import concourse.bass as bass
import concourse.mybir as mybir
from concourse.bass_utils import run_bass_kernel_spmd

F32 = mybir.dt.float32
BF16 = mybir.dt.bfloat16
I32 = mybir.dt.int32
AF = mybir.ActivationFunctionType
ALU = mybir.AluOpType
AX = mybir.AxisListType

SEM_LIMIT = 30000
STRICT = True
RELAXED = ("pe",)


class Buf:
    __slots__ = ("lw", "rd", "dsem", "dval", "name", "excl")

    def __init__(self, name="", excl=False):
        self.excl = excl
        self.lw = None
        self.rd = []
        self.dsem = None
        self.dval = 0
        self.name = name


class V:
    __slots__ = ("ap", "bufs")

    def __init__(self, ap, bufs):
        self.ap = ap
        self.bufs = bufs

    def __getitem__(self, idx):
        return V(self.ap[idx], self.bufs)

    def with_bufs(self, bufs):
        return V(self.ap, bufs)


class Eng:
    def __init__(self, K, name, h):
        self.K = K
        self.name = name
        self.h = h
        self.n = 0
        self.last = None
        self.sig_idx = []
        self.sig_tok = []
        self.sem = None
        self.semval = 0
        self.known = {}
        self.known_dma = {}

    def signal_for(self, idx):
        p = bisect.bisect_left(self.sig_idx, idx)
        if p < len(self.sig_idx):
            return self.sig_idx[p], self.sig_tok[p]
        assert self.n - 1 >= idx and self.last is not None
        if self.sem is None or self.semval >= SEM_LIMIT:
            self.sem = self.K.new_sem(self.name)
            self.semval = 0
        self.semval += 1
        self.last.then_inc(self.sem, 1)
        self.sig_idx.append(self.n - 1)
        self.sig_tok.append((self.sem, self.semval))
        return self.n - 1, (self.sem, self.semval)


class Kern:
    def __init__(self, num_devices=None):
        if num_devices is None:
            self.nc = bass.Bass("TRN2", target_bir_lowering=False)
        else:
            self.nc = bass.Bass("TRN2", target_bir_lowering=False, num_devices=num_devices)
        self.stack = ExitStack()
        nc = self.nc
        self.eng = {
            "pe": Eng(self, "pe", nc.tensor),
            "act": Eng(self, "act", nc.scalar),
            "dve": Eng(self, "dve", nc.vector),
            "pool": Eng(self, "pool", nc.gpsimd),
            "sp": Eng(self, "sp", nc.sync),
        }
        self.nsem = 0
        self.out_toks = []
        self.ninstr = 0
        self.defer = None

    def new_sem(self, name):
        self.nsem += 1
        return self.stack.enter_context(self.nc.semaphore(f"s{self.nsem}_{name}"))

    def dram(self, name, shape, dtype, kind):
        t = self.nc.dram_tensor(name, list(shape), dtype, kind=kind)
        return V(t.ap(), [Buf(name)])

    def sbuf(self, name, shape, dtype, nbufs=1):
        t = self.stack.enter_context(self.nc.sbuf_tensor(name, list(shape), dtype))
        return V(t[:], [Buf(name)])

    def psum(self, name, shape, dtype=F32):
        t = self.stack.enter_context(self.nc.psum_tensor(name, list(shape), dtype))
        return V(t[:], [Buf(name, excl=True)])

    def _wait_tok(self, F, tok, same_engine_ok):
        if tok is None:
            return
        if tok[0] == "e":
            _, E, idx = tok
            if E is F and same_engine_ok and (F.name in RELAXED or not STRICT):
                return
            if F.known.get(E.name, -1) >= idx:
                return
            sidx, (sem, val) = E.signal_for(idx)
            F.h.wait_ge(sem, val)
            F.known[E.name] = sidx
        else:
            _, sem, val, sid = tok
            if F.known_dma.get(sid, 0) >= val:
                return
            F.h.wait_ge(sem, val)
            F.known_dma[sid] = val

    def _deps(self, F, reads, writes):
        for v in reads:
            for b in v.bufs:
                self._wait_tok(F, b.lw, False)
                if b.excl:
                    for t in b.rd:
                        if t[0] == "e" and t[1] is not F:
                            self._wait_tok(F, t, True)
        for v in writes:
            for b in v.bufs:
                self._wait_tok(F, b.lw, True)
                for t in b.rd:
                    self._wait_tok(F, t, True)

    def _commit(self, tok, reads, writes):
        for v in reads:
            for b in v.bufs:
                b.rd.append(tok)
                if len(b.rd) > 24:
                    b.rd = b.rd[-24:] if False else b.rd
        for v in writes:
            for b in v.bufs:
                b.lw = tok
                b.rd = []

    def op(self, ename, fn, reads, writes):
        if self.defer is not None:
            self.defer.append(("op", ename, fn, reads, writes))
            return None
        F = self.eng[ename]
        self._deps(F, reads, writes)
        ins = fn()
        F.last = ins
        idx = F.n
        F.n += 1
        self.ninstr += 1
        self._commit(("e", F, idx), reads, writes)
        return ins

    def replay(self, items):
        for it in items:
            if it[0] == "op":
                self.op(it[1], it[2], it[3], it[4])
            else:
                tok = self.dma(it[1], it[2], it[3], owner=it[4])
                if it[5] is not None:
                    it[5](tok)

    def dma(self, qname, out, in_, owner=None, cb=None):
        if self.defer is not None:
            self.defer.append(("dma", qname, out, in_, owner, cb))
            return None
        F = self.eng[qname]
        self._deps(F, [in_], [out])
        if owner is None:
            owner = out.bufs[0]
        if owner.dsem is None:
            owner.dsem = self.new_sem("d")
            owner.dval = 0
        if owner.dval + 16 > SEM_LIMIT * 2:
            raise RuntimeError("dma sem overflow " + str(owner.name))
        owner.dval += 16
        ins = F.h.dma_start(out=out.ap, in_=in_.ap)
        ins.then_inc(owner.dsem, 16)
        self.ninstr += 1
        tok = ("d", owner.dsem, owner.dval, id(owner))
        self._commit(tok, [in_], [out])
        return tok

    def allgather(self, out, in_, ranks):
        F = self.eng["pool"]
        self._deps(F, [in_], [out])
        owner = out.bufs[0]
        if owner.dsem is None:
            owner.dsem = self.new_sem("cc")
            owner.dval = 0
        owner.dval += 16
        ins = self.nc.gpsimd.collective_compute("AllGather", ALU.bypass, replica_groups=[list(range(ranks))],
                                                ins=[in_.ap], outs=[out.ap])
        ins.then_inc(owner.dsem, 16)
        self.ninstr += 1
        tok = ("d", owner.dsem, owner.dval, id(owner))
        self._commit(tok, [in_], [out])
        return tok

    def finish(self, toks):
        F = self.eng["sp"]
        for t in toks:
            self._wait_tok(F, t, False)

    def mm(self, out, lhsT, rhs, start=True, stop=True, skip=False):
        return self.op("pe", lambda: self.nc.tensor.matmul(out.ap, lhsT.ap, rhs.ap, start=start, stop=stop,
                                                           skip_group_check=skip),
                       [lhsT, rhs], [out])

    def transpose(self, out, in_, ident):
        return self.op("pe", lambda: self.nc.tensor.transpose(out.ap, in_.ap, ident.ap), [in_, ident], [out])

    def act(self, out, in_, func, bias=None, scale=1.0, accum_out=None):
        reads = [in_]
        kw = {}
        if isinstance(bias, V):
            reads.append(bias); kw["bias"] = bias.ap
        elif bias is not None:
            kw["bias"] = bias
        if isinstance(scale, V):
            reads.append(scale); kw["scale"] = scale.ap
        else:
            kw["scale"] = scale
        writes = [out]
        if accum_out is not None:
            writes.append(accum_out); kw["accum_out"] = accum_out.ap
        return self.op("act", lambda: self.nc.scalar.activation(out=out.ap, in_=in_.ap, func=func, **kw), reads, writes)

    def _e(self, eng):
        return {"dve": self.nc.vector, "pool": self.nc.gpsimd, "act": self.nc.scalar}[eng]

    def copy(self, out, in_, eng="dve"):
        if eng == "act":
            return self.op("act", lambda: self.nc.scalar.copy(out=out.ap, in_=in_.ap), [in_], [out])
        return self.op(eng, lambda: self._e(eng).tensor_copy(out.ap, in_.ap), [in_], [out])

    def tt(self, out, in0, in1, op, eng="dve"):
        return self.op(eng, lambda: self._e(eng).tensor_tensor(out.ap, in0.ap, in1.ap, op), [in0, in1], [out])

    def ts(self, out, in0, s1, op0, s2=None, op1=None, eng="dve", accum_out=None):
        reads = [in0]
        a1 = s1.ap if isinstance(s1, V) else s1
        a2 = s2.ap if isinstance(s2, V) else s2
        if isinstance(s1, V): reads.append(s1)
        if isinstance(s2, V): reads.append(s2)
        writes = [out]
        kw = {}
        if accum_out is not None:
            writes.append(accum_out); kw["accum_out"] = accum_out.ap
        if op1 is None:
            return self.op(eng, lambda: self._e(eng).tensor_scalar(out.ap, in0.ap, a1, None, op0, **kw), reads, writes)
        return self.op(eng, lambda: self._e(eng).tensor_scalar(out.ap, in0.ap, a1, a2, op0, op1, **kw), reads, writes)

    def stt(self, out, in0, s, in1, op0, op1):
        reads = [in0, in1]
        a = s.ap if isinstance(s, V) else s
        if isinstance(s, V): reads.append(s)
        return self.op("dve", lambda: self.nc.vector.scalar_tensor_tensor(out.ap, in0.ap, a, in1.ap, op0, op1), reads, [out])

    def scan(self, out, d0, d1, initial, op0, op1):
        reads = [d0, d1]
        a = initial.ap if isinstance(initial, V) else initial
        if isinstance(initial, V): reads.append(initial)
        return self.op("dve", lambda: self.nc.vector.tensor_tensor_scan(out.ap, d0.ap, d1.ap, a, op0, op1), reads, [out])

    def memset(self, out, val, eng="dve"):
        return self.op(eng, lambda: self._e(eng).memset(out.ap, val), [], [out])

    def recip(self, out, in_):
        return self.op("dve", lambda: self.nc.vector.reciprocal(out.ap, in_.ap), [in_], [out])

    def reduce(self, out, in_, op, axis=AX.X):
        return self.op("dve", lambda: self.nc.vector.tensor_reduce(out.ap, in_.ap, axis, op), [in_], [out])

import math
import numpy as np

G = 512
NT = 4
C = 64
HG = 256
NCH = HG // C
OFF_A, OFF_B, OFF_C, OFF_D, OFF_T1, OFF_MIX = 0, 128, 256, 384, 512, 770
N_MIX = 448
OFF_E, OFF_F, OFF_GD, OFF_T2 = 0, 128, 256, 384
NCOL = OFF_MIX + N_MIX
NCOLB = OFF_MIX + 2 * N_MIX
LWC = 0.6065306597126334


def vr(v, pat, **kw):
    return V(v.ap.rearrange(pat, **kw), v.bufs)


def vb(v, shape):
    return V(v.ap.broadcast_to(list(shape)), v.bufs)


def build_A(S):
    NG = S // G
    NTT = S // 128
    K = Kern()
    xT = K.dram("xT", [1024, S], F32, "ExternalInput")
    W = K.dram("W", [1024, NCOL], F32, "ExternalInput")
    mu = K.dram("mu", [1, N_MIX], F32, "ExternalInput")
    pcol = K.dram("pcol", [64, 8], F32, "ExternalInput")
    w2h = K.dram("w2h", [64, 64], F32, "ExternalInput")
    a2h = K.dram("a2h", [64, 64], F32, "ExternalInput")
    g2h = K.dram("g2h", [128, 64], F32, "ExternalInput")
    prow = K.dram("prow", [1, 194], F32, "ExternalInput")
    cst = K.dram("cst", [128, 512], F32, "ExternalInput")
    cst64 = K.dram("cst64", [64, 192], F32, "ExternalInput")
    rst = K.dram("rst", [64, HG], F32, "ExternalInput")
    rot = K.dram("rot", [64, 2, S], F32, "ExternalInput")
    retc = K.dram("retc", [128, 642], F32, "ExternalInput")
    y = K.dram("y", [S, 256], F32, "ExternalOutput")

    sb = K.sbuf
    wb = sb("wb", [128, 8, NCOLB], BF16)
    stg = [sb(f"stg{i}", [128, 1280], F32) for i in range(2)]
    mub = sb("mub", [128, N_MIX], F32)
    wtmp = sb("wtmp", [128, N_MIX], F32)
    pc = sb("pc", [64, 8], F32)
    w2b = sb("w2b", [64, 64], BF16); a2b = sb("a2b", [64, 64], BF16); g2b = sb("g2b", [128, 64], BF16)
    prb = sb("prb", [128, 194], F32)
    cs = sb("cs", [128, 512], F32)
    cs64 = sb("cs64", [64, 192], F32)
    rstm = sb("rstm", [64, HG], F32)
    rc = sb("rc", [128, 642], F32)
    causal = sb("causal", [128, 128], BF16)
    c8 = sb("c8", [1, 128], BF16)
    ident = cs[:, 0:128]; ucum = cs[:, 128:256]; ones = cs[:, 256:384]
    I64 = cs[0:64, 0:64]

    def m3(i):
        return vb(vr(cs64[:, 64 * i:64 * i + 64], "p (o t) -> p o t", o=1), [64, NCH, C])
    mUs, mLs, mUi = m3(0), m3(1), m3(2)
    I3 = vb(vr(I64, "p (o t) -> p o t", o=1), [64, NCH, C])

    xb = [sb(f"xb{i}", [128, 8, G + 1], BF16) for i in range(2)]
    rott = sb("rott", [64, 2, G], F32)

    Kc = sb("Kc", [128, S], BF16)
    Vc = sb("Vc", [128, NTT, 2, 65], BF16)
    kbufs = [Buf(f"kg{g}") for g in range(NG)]
    vbufs = [Buf(f"vg{g}") for g in range(NG)]
    ctab = sb("ctab", [128, 2, NTT], F32)
    cbufs = [Buf(f"cg{g}") for g in range(NG)]
    carry = [sb(f"carry{i}", [128, 2], F32) for i in range(2)]
    Qa = sb("Qa", [128, G], BF16)
    rqrow = [sb(f"rqrow{h}", [1, G], BF16) for h in range(2)]
    spg = sb("spg", [128, NT, 2], F32)
    spe = sb("spe", [128, NT, 2], F32)
    win = sb("win", [128, 2, NT], F32)
    inc = sb("inc", [128, 2, NT], F32)
    rq = sb("rq", [128, 2, NT], F32)
    biasg = [sb(f"biasg{h}", [128, NTT], F32) for h in range(2)]
    pts = [sb(f"pt{i}", [128, G], BF16) for i in range(3)]
    rcp = sb("rcp", [128, NT], F32)
    yfox = sb("yfox", [128, NT, 128], F32)

    P = [K.psum(f"P{i}", [128, 512], F32) for i in range(8)]

    w2f = stg[0][0:64, 0:64]; a2f = stg[0][0:64, 64:128]; g2f = stg[0][:, 128:192]
    K.dma("sp", pc, pcol)
    K.dma("sp", w2f, w2h); K.dma("sp", a2f, a2h); K.dma("sp", g2f, g2h)
    K.dma("sp", cs, cst); K.dma("sp", cs64, cst64); K.dma("sp", rstm, rst); K.dma("sp", rc, retc)
    K.dma("sp", mub, V(mu.ap.partition_broadcast(128), mu.bufs))
    K.dma("sp", prb, V(prow.ap.partition_broadcast(128), prow.bufs))
    K.copy(w2b, w2f); K.copy(a2b, a2f); K.copy(g2b, g2f)
    K.copy(causal, cs[:, 384:512])
    K.memset(c8, 8.0)
    K.ts(pc[:, 4:5], pc[:, 3:4], -1.0, ALU.mult, 1.0, ALU.add)
    Wv = vr(W, "(kt p) c -> kt p c", p=128)
    for kt in range(8):
        st = stg[kt % 2]
        K.dma("sp", st[:, 0:NCOL], Wv[kt])
        K.copy(wb[:, kt, 0:OFF_MIX], st[:, 0:OFF_MIX], eng="act")
        K.tt(wtmp, st[:, OFF_MIX:NCOL], mub, ALU.mult)
        K.copy(wb[:, kt, OFF_MIX + N_MIX:NCOLB], wtmp, eng="pool")
        K.tt(wb[:, kt, OFF_MIX:OFF_MIX + N_MIX], st[:, OFF_MIX:NCOL], wtmp, ALU.subtract)
    K.memset(Vc[:, :, :, 64:65].with_bufs(vbufs), 1.0)
    K.memset(carry[0], 0.0)
    K.memset(xb[1][:, :, G:G + 1], 0.0)

    xTv = vr(xT, "(kt p) s -> p kt s", p=128)

    def load_x(g, q):
        st = stg[q % 2]
        sv = vr(st[:, 0:1024], "p (k s) -> p k s", k=2)
        K.dma("sp", sv, xTv[:, 2 * q:2 * q + 2, g * G:(g + 1) * G])
        K.copy(xb[g % 2][:, 2 * q:2 * q + 2, 1:G + 1], sv, eng=("act" if q % 2 == 0 else "dve"))

    out_toks = []

    ST = [sb(f"ST{i}", [64, 64], F32) for i in range(2)]
    K.memset(ST[0], 0.0)
    Rst = [sb(f"Rst{i}", [64, 64], F32) for i in range(2)]
    Rb = [sb(f"Rb{i}", [64, 64], BF16) for i in range(2)]
    K.memset(Rst[0], 0.0); K.memset(Rb[0], 0.0)
    st_idx = [0]
    r_idx = [0]

    A = [sb(f"A{i}", [64, HG], F32) for i in range(28)]
    thw = sb("thw", [64, HG], BF16); adb = sb("adb", [64, HG], BF16); sgd = sb("sgd", [128, HG], BF16)
    vtok = sb("vtok", [64, 2 * NCH, 64], F32)
    st1 = sb("st1", [64, NCH], F32); st2 = sb("st2", [64, NCH], F32); rkv = sb("rkv", [64, NCH], F32)
    yrwo = [sb(f"yrwo{i}", [64, NCH, 64], F32) for i in range(2)]
    qrT = sb("qrT", [64, G], BF16); krT = sb("krT", [64, G], BF16); qdT = sb("qdT", [64, G], BF16)
    krTf = sb("krTf", [64, G], F32)
    vret = sb("vret", [128, NT, 64], BF16)
    gret = sb("gret", [128, NT, 64], F32)
    sTr = [sb(f"sTr{i}", [128, 128], BF16) for i in range(2)]
    kdt = [sb(f"kdt{i}", [128, 64], BF16) for i in range(2)]
    yr = sb("yr", [128, NT, 64], F32); yrc = sb("yrc", [128, NT, 64], F32); yrs = sb("yrs", [128, NT, 64], F32)
    rs1 = sb("rs1", [128, NT], F32); rs2 = sb("rs2", [128, NT], F32)
    yreto = sb("yreto", [128, NT, 64], F32)

    def groupnorm(yv, ycv, ysqv, s1, s2, eps, Pn, n):
        K.reduce(s1, yv, ALU.add)
        K.ts(s1, s1, -1.0 / 64, ALU.mult)
        K.tt(ycv, yv, vb(vr(s1, "p (n o) -> p n o", o=1), [Pn, n, 64]), ALU.add)
        K.tt(ysqv, ycv, ycv, ALU.mult)
        K.reduce(s2, ysqv, ALU.add)
        K.ts(s2, s2, 1.0 / 64, ALU.mult, float(eps), ALU.add)
        K.act(s2, s2, AF.Ln)
        K.act(s2, s2, AF.Exp, scale=-0.5)
        K.tt(ycv, ycv, vb(vr(s2, "p (n o) -> p n o", o=1), [Pn, n, 64]), ALU.mult)

    def c3(v):
        return vr(v, "p (c t) -> p c t", c=NCH)

    def ch(v, c):
        return v[:, c * C:(c + 1) * C]

    for q in range(4):
        load_x(0, q)

    for g in range(NG):
        cur, prv = g % 2, (g + 1) % 2
        t0 = g * G
        K.copy(xb[cur][:, :, 0:1], xb[prv][:, :, G:G + 1], eng="pool")
        X = xb[cur]
        K.dma("sp", rott, rot[:, :, t0:t0 + G])

        def proj_fm(pout, off, mixed, c0=0, n=G):
            for kt in range(8):
                if not mixed:
                    K.mm(pout, wb[:, kt, off:off + 128], X[:, kt, 1 + c0:1 + c0 + n], start=(kt == 0), stop=(kt == 7))
                else:
                    K.mm(pout, wb[:, kt, OFF_MIX + off:OFF_MIX + off + 128], X[:, kt, 1 + c0:1 + c0 + n], start=(kt == 0), stop=False)
                    K.mm(pout, wb[:, kt, OFF_MIX + N_MIX + off:OFF_MIX + N_MIX + off + 128], X[:, kt, c0:c0 + n], start=False, stop=(kt == 7))

        proj_fm(P[0], OFF_A, False)
        K.copy(Qa, P[0], eng="act")
        proj_fm(P[1], OFF_B, False)
        K.copy(Kc[:, t0:t0 + G].with_bufs([kbufs[g]]), P[1], eng="dve")
        for tt_ in range(NT):
            pb = tt_ % 2
            tsl = slice(1 + tt_ * 128, 1 + tt_ * 128 + 128)
            tsl_prev = slice(tt_ * 128, tt_ * 128 + 128)
            for kt in range(8):
                K.mm(P[pb][:, 0:258], X[:, kt, tsl], wb[:, kt, OFF_T1:OFF_T1 + 258], start=(kt == 0), stop=(kt == 7))
            for kt in range(8):
                K.mm(P[pb][:, 258:322], X[:, kt, tsl], wb[:, kt, OFF_MIX + OFF_T2:OFF_MIX + OFF_T2 + 64], start=(kt == 0), stop=False)
                K.mm(P[pb][:, 258:322], X[:, kt, tsl_prev], wb[:, kt, OFF_MIX + N_MIX + OFF_T2:OFF_MIX + N_MIX + OFF_T2 + 64], start=False, stop=(kt == 7))
            tile = g * NT + tt_
            K.copy(Vc[:, tile, :, 0:64].with_bufs([vbufs[g]]), vr(P[pb][:, 0:128], "p (h d) -> p h d", h=2), eng="act")
            K.tt(spg[:, tt_, :], P[pb][:, 128:130], prb[:, 0:2], ALU.add)
            K.copy(vret[:, tt_, :], P[pb][:, 130:194], eng="dve")
            K.act(gret[:, tt_, :], P[pb][:, 194:258], AF.Silu)
            for hh in range(2):
                K.copy(vtok[:, tt_ * 2 + hh, :], P[pb][64 * hh:64 * hh + 64, 258:322], eng="dve")
        K.act(spe, spg, AF.Exp, scale=-1.0)
        K.ts(spe, spe, 1.0, ALU.add)
        K.act(spe, spe, AF.Ln)
        spv = vr(spe, "p t h -> p (t h)")
        K.mm(P[0][:, 0:8], ucum, spv)
        K.mm(P[0][:, 8:16], ones, spv)
        pv4 = vr(P[0][:, 0:16], "p (a t h) -> p a h t", a=2, h=2)
        K.copy(win, pv4[:, 0], eng="dve")
        K.copy(rq, pv4[:, 1], eng="dve")
        for h in range(2):
            K.scan(inc[:, h, :], ones[:, 0:NT], rq[:, h, :], carry[cur][:, h:h + 1], ALU.mult, ALU.add)
        K.tt(win, win, inc, ALU.add)
        K.tt(ctab[:, :, g * NT:(g + 1) * NT].with_bufs([cbufs[g]]), win, rq, ALU.subtract)
        K.copy(carry[prv], inc[:, :, NT - 1], eng="dve")
        K.tt(rq, vb(inc[:, :, NT - 1:NT], [128, 2, NT]), inc, ALU.subtract)
        nk = (g + 1) * NT
        for h in range(2):
            K.copy(vr(rqrow[h], "p (j t) -> p j t", j=NT),
                   vb(vr(rq[0:1, h, :], "p (j o) -> p j o", o=1), [1, NT, 128]), eng="dve")
            K.ts(biasg[h][:, 0:nk], ctab[:, h, 0:nk].with_bufs(cbufs[0:g + 1]), inc[:, h, NT - 1:NT], ALU.subtract)
        if g + 1 < NG:
            for q in range(4):
                load_x(g + 1, q)
        K.defer = []
        for (off, pbk, dst, dstf) in ((OFF_C, 0, qrT, None), (OFF_D, 1, krT, krTf)):
            proj_fm(P[pbk], off, False)
            for hf in range(2):
                hsl = slice(hf * HG, (hf + 1) * HG)
                K.tt(A[0], P[pbk][0:64, hsl], rott[:, 0, hsl], ALU.mult)
                K.tt(A[1], P[pbk][64:128, hsl], rott[:, 1, hsl], ALU.mult)
                if dstf is None:
                    K.tt(dst[:, hsl], A[0], A[1], ALU.add)
                else:
                    K.tt(dstf[:, hsl], A[0], A[1], ALU.add)
                    K.copy(dst[:, hsl], dstf[:, hsl], eng="act")
        K.tt(qdT, qrT, rc[0:64, 129:641], ALU.mult)
        Yr = vr(P[6][:, 0:NT * 64], "p (j e) -> p j e", j=NT)
        for n in range(NT):
            csl = slice(n * 128, (n + 1) * 128)
            K.mm(P[7][:, 0:128], krT[:, csl], qrT[:, csl])
            sT = sTr[n % 2]
            K.tt(sT, P[7][:, 0:128], rc[:, 0:128], ALU.mult)
            K.transpose(P[7][:, 256:320], krTf[:, csl], I64)
            kd = kdt[n % 2]
            K.ts(kd, P[7][:, 256:320], rc[:, 128:129], ALU.mult)
            ri = r_idx[0]
            K.mm(Yr[:, n, :], sT, vret[:, n, :], start=True, stop=False)
            K.mm(Yr[:, n, :], qdT[:, csl], Rb[ri], start=False, stop=True)
            K.mm(P[7][0:64, 384:448], kd, vret[:, n, :])
            K.stt(Rst[1 - ri], Rst[ri], rc[0:64, 641:642], P[7][0:64, 384:448], ALU.mult, ALU.add)
            K.copy(Rb[1 - ri], Rst[1 - ri], eng="act")
            r_idx[0] = 1 - ri
        K.copy(yr, Yr, eng="act")
        groupnorm(yr, yrc, yrs, rs1, rs2, 1e-6, 128, NT)
        K.tt(yrc, yrc, vb(vr(prb[:, 130:194], "p (o e) -> p o e", o=1), [128, NT, 64]), ALU.mult)
        K.tt(yreto, yrc, gret, ALU.mult)
        yo = V(y.ap[t0:t0 + G, 192:256].rearrange("(j p) c -> p j c", p=128), [Buf()])
        K.dma("pool", yo, yreto, owner=yreto.bufs[0], cb=out_toks.append)

        for hf in range(2):
            c0 = hf * HG
            (rT, kT, sig, aT, kk, sq, rn, bT, ktl, tmpa, csum, epos, eneg, eprev,
             KpT, BpT, KtT, RpT, BppT, KtppT, Dg, rkr, Pm, PTm, AbrT, AkrT, gtok, spare) = A
            QtT, nLkV, Um, Wm, Gm, HT, yrw = rT, kT, sig, aT, kk, sq, rn
            Kptok, Bpptok, Ktpptok, yc_, ysq, LkTm, MT = bT, ktl, tmpa, csum, epos, eneg, eprev
            p0 = P[0][0:64, 0:HG]; p1 = P[1][0:64, 0:HG]
            proj_fm(P[0][:, 0:HG], OFF_E, True, c0, HG)
            K.copy(rT, P[0][0:64, 0:HG], eng="act")
            K.copy(kT, P[0][64:128, 0:HG], eng="dve")
            proj_fm(P[1][:, 0:HG], OFF_F, True, c0, HG)
            K.act(thw, P[1][0:64, 0:HG], AF.Tanh)
            K.copy(adb, P[1][64:128, 0:HG], eng="dve")
            proj_fm(P[0][:, 0:HG], OFF_GD, True, c0, HG)
            K.act(sgd, P[0][:, 0:HG], AF.Sigmoid)
            K.mm(p1, w2b, thw)
            K.act(sig, p1, AF.Sigmoid, bias=pc[:, 0:1])
            K.mm(p0, a2b, adb)
            K.act(aT, p0, AF.Sigmoid, bias=pc[:, 1:2])
            for t2 in range(2):
                K.mm(P[1][:, 256:320], sgd[:, t2 * 128:(t2 + 1) * 128], g2b)
                for hh in range(2):
                    cch = t2 * 2 + hh
                    K.copy(ch(gtok, cch), P[1][64 * hh:64 * hh + 64, 256:320], eng="act")
            K.ts(kk, kT, pc[:, 2:3], ALU.mult)
            K.tt(sq, kk, kk, ALU.mult)
            K.mm(p0, ones[0:64, 0:64], sq)
            K.act(rn, p0, AF.Sqrt)
            K.ts(rn, rn, 1e-12, ALU.max)
            K.recip(rn, rn)
            K.tt(kk, kk, rn, ALU.mult)
            K.tt(bT, kk, aT, ALU.mult)
            K.ts(tmpa, aT, pc[:, 3:4], ALU.mult, pc[:, 4:5], ALU.add)
            K.tt(ktl, kT, tmpa, ALU.mult)
            K.tt(rkr, rT, ktl, ALU.mult)
            K.ts(rkr, rkr, pc[:, 5:6], ALU.mult)
            K.scan(csum, rstm, sig, 0.0, ALU.mult, ALU.add)
            K.act(epos, csum, AF.Exp, scale=-LWC)
            K.act(eneg, csum, AF.Exp, scale=LWC)
            K.tt(tmpa, csum, sig, ALU.subtract)
            K.act(eprev, tmpa, AF.Exp, scale=-LWC)
            K.tt(KpT, kk, eprev, ALU.mult)
            K.tt(BpT, bT, eneg, ALU.mult)
            K.tt(KtT, ktl, eneg, ALU.mult)
            K.tt(RpT, rT, epos, ALU.mult)
            eCb = vb(c3(epos)[:, :, C - 1:C], [64, NCH, C])
            K.tt(c3(BppT), c3(BpT), eCb, ALU.mult)
            K.tt(c3(KtppT), c3(KtT), eCb, ALU.mult)
            K.tt(c3(Dg), I3, eCb, ALU.mult)

            def pcm(pout, lhs, rhs):
                for c in range(NCH):
                    K.mm(ch(pout, c), ch(lhs, c), ch(rhs, c))
            pcm(p0, BpT, KpT)
            K.stt(c3(PTm), c3(p0), -1.0, mUs, ALU.mult, ALU.mult)
            pcm(p1, KpT, BpT)
            K.stt(c3(Pm), c3(p1), -1.0, mLs, ALU.mult, ALU.mult)
            pcm(p0, KtT, KpT)
            K.tt(c3(LkTm), c3(p0), mUs, ALU.mult)
            pcm(p1, BpT, RpT)
            K.tt(c3(AbrT), c3(p1), mUi, ALU.mult)
            pcm(p0, KtT, RpT)
            K.tt(c3(AkrT), c3(p0), mUi, ALU.mult)
            K.tt(c3(MT), c3(PTm), I3, ALU.add)
            for lvl in range(1, 6):
                pcm(p0, PTm, Pm)
                pcm(p1, Pm, PTm)
                K.copy(Pm, p0, eng="act")
                K.copy(PTm, p1, eng="dve")
                pcm(p0, Pm, MT)
                K.tt(MT, MT, p0, ALU.add)
            for src, dst, pp in ((KpT, Kptok, p0), (BppT, Bpptok, p1), (KtppT, Ktpptok, p0)):
                for c in range(NCH):
                    K.transpose(ch(pp, c), ch(src, c), I64)
                K.copy(dst, pp, eng=("act" if pp is p0 else "dve"))
            vh = vr(vtok[:, hf * NCH:(hf + 1) * NCH, :], "p c e -> p (c e)")
            pcm(p1, LkTm, vh)
            K.ts(nLkV, p1, -1.0, ALU.mult)
            pcm(p0, MT, nLkV)
            K.copy(Um, p0, eng="act")
            pcm(p1, MT, Kptok)
            K.copy(Wm, p1, eng="dve")
            pcm(p0, Wm, Bpptok)
            K.tt(Gm, Dg, p0, ALU.subtract)
            for c in range(NCH):
                K.mm(ch(p1, c), ch(Bpptok, c), ch(Um, c), start=True, stop=False)
                K.mm(ch(p1, c), ch(Ktpptok, c), ch(vh, c), start=False, stop=True)
            K.copy(HT, p1, eng="act")
            pcm(p0, Wm, AbrT)
            K.tt(QtT, RpT, p0, ALU.subtract)
            for c in range(NCH):
                K.mm(P[1][0:64, 448 + c:449 + c], ch(rkr, c), ones[0:64, 0:1])
            K.copy(rkv, P[1][0:64, 448:448 + NCH], eng="dve")
            Yp = P[6][0:64, 256:256 + HG]
            for c in range(NCH):
                si = st_idx[0]
                K.mm(ch(Yp, c), ch(QtT, c), ST[si], start=True, stop=False)
                K.mm(ch(Yp, c), ch(AbrT, c), ch(Um, c), start=False, stop=False)
                K.mm(ch(Yp, c), ch(AkrT, c), ch(vh, c), start=False, stop=True)
                K.mm(P[7][0:64, 448:512], ch(Gm, c), ST[si])
                K.tt(ST[1 - si], P[7][0:64, 448:512], ch(HT, c), ALU.add)
                st_idx[0] = 1 - si
            K.copy(yrw, Yp, eng="act")
            groupnorm(c3(yrw), c3(yc_), c3(ysq), st1, st2, 64e-5, 64, NCH)
            K.tt(c3(yc_), c3(yc_), vb(vr(prb[0:64, 2:66], "p (o e) -> p o e", o=1), [64, NCH, 64]), ALU.mult)
            K.tt(c3(yc_), c3(yc_), vb(vr(prb[0:64, 66:130], "p (o e) -> p o e", o=1), [64, NCH, 64]), ALU.add)
            K.tt(c3(ysq), c3(vh), vb(vr(rkv, "p (c o) -> p c o", o=1), [64, NCH, 64]), ALU.mult)
            K.tt(yc_, yc_, ysq, ALU.add)
            yo_t = yrwo[hf]
            K.tt(vr(yo_t, "p c e -> p (c e)"), yc_, gtok, ALU.mult)
            yo = V(y.ap[t0 + c0:t0 + c0 + HG, 128:192].rearrange("(c p) e -> p c e", p=C), [Buf()])
            K.dma("pool", yo, yo_t, owner=yo_t.bufs[0], cb=out_toks.append)

        deferred = K.defer
        K.defer = None
        pti = 0
        n_iter = 2 * nk
        per = max(1, -(-len(deferred) // n_iter))
        dpos = 0
        for h in range(2):
            hs = slice(64 * h, 64 * h + 64)
            PV = vr(P[4][:, 0:NT * 65], "p (j e) -> p j e", j=NT)
            SB3 = (P[2], P[3], P[5])
            def emit_st(kt):
                j = kt - g * NT
                q0 = 128 * max(j, 0)
                ncol = G - q0
                kg = kt // NT
                K.mm(SB3[kt % 3][:, 0:ncol], Kc[hs, kt * 128:(kt + 1) * 128].with_bufs([kbufs[kg]]), Qa[hs, q0:G], start=True, stop=False)
                K.mm(SB3[kt % 3][:, 0:ncol], c8, rqrow[h][:, q0:G], start=False, stop=True)
            def emit_exp(kt):
                j = kt - g * NT
                ncol = G - 128 * max(j, 0)
                pt = pts[kt % 3]
                K.act(pt[:, 0:ncol], SB3[kt % 3][:, 0:ncol], AF.Exp, bias=biasg[h][:, kt:kt + 1], scale=0.125)
                if j >= 0:
                    K.tt(pt[:, 0:128], pt[:, 0:128], causal, ALU.mult, eng="pool")
            emit_st(0)
            if nk > 1:
                emit_st(1)
            emit_exp(0)
            for kt in range(nk):
                if kt + 2 < nk:
                    emit_st(kt + 2)
                if kt + 1 < nk:
                    emit_exp(kt + 1)
                j = kt - g * NT
                kg = kt // NT
                pt = pts[kt % 3]
                for jj in range(max(j, 0), NT):
                    cc0 = (jj - max(j, 0)) * 128
                    K.mm(PV[:, jj, :], pt[:, cc0:cc0 + 128], Vc[:, kt, h, :].with_bufs([vbufs[kg]]),
                         start=(kt == 0 and jj == 0), stop=(kt == g * NT + jj), skip=True)
                K.replay(deferred[dpos:dpos + per]); dpos += per
            K.recip(rcp, PV[:, :, 64])
            K.tt(yfox[:, :, 64 * h:64 * h + 64], PV[:, :, 0:64], vb(vr(rcp, "p (j o) -> p j o", o=1), [128, NT, 64]), ALU.mult)
        yo = V(y.ap[t0:t0 + G, 0:128].rearrange("(j p) c -> p j c", p=128), [Buf()])
        out_toks.append(K.dma("pool", yo, yfox, owner=yfox.bufs[0]))
        K.replay(deferred[dpos:])


    K.finish(out_toks)
    return K


def host_consts(S, head):
    ident = np.eye(128, dtype=np.float32)
    ucum = np.triu(np.ones((128, 128), np.float32))
    ones = np.ones((128, 128), np.float32)
    causal = np.triu(np.ones((128, 128), np.float32))
    cst = np.concatenate([ident, ucum, ones, causal], 1)
    mUs = np.triu(np.ones((64, 64), np.float32), 1)
    mLs = np.tril(np.ones((64, 64), np.float32), -1)
    mUi = np.triu(np.ones((64, 64), np.float32), 0)
    cst64 = np.concatenate([mUs, mLs, mUi], 1)
    rst = np.ones((64, HG), np.float32); rst[:, ::64] = 0.0
    half = 32
    inv = 10000.0 ** (-np.arange(half, dtype=np.float64) / half)
    inv = inv.astype(np.float32).astype(np.float64)
    pos = np.arange(S, dtype=np.float64)
    ang = (pos[None, :].astype(np.float32) * inv[:, None].astype(np.float32)).astype(np.float64)
    cos, sin = np.cos(ang), np.sin(ang)
    CC = np.concatenate([cos, cos], 0)
    SS = np.concatenate([-sin, sin], 0)
    rot = np.ascontiguousarray(np.stack([CC, SS], 1).astype(np.float32))
    lg = np.log1p(-np.exp2(-5.0 - head))
    idx = np.arange(128, dtype=np.float64)
    diff = idx[None, :] - idx[:, None]
    dmT = np.where(diff >= 0, np.exp(np.maximum(diff, 0) * lg), 0.0) * 0.125
    kdec = np.exp((127.0 - idx) * lg) * 0.125
    qdec = np.exp((idx + 1.0) * lg)
    retc = np.zeros((128, 642), np.float32)
    retc[:, 0:128] = dmT
    retc[:, 128] = kdec
    retc[:, 129:641] = np.tile(qdec, 4)[None, :]
    retc[:, 641] = np.exp(128 * lg)
    return dict(cst=cst, cst64=cst64, rst=rst, rot=rot, retc=retc)


IN_SPL = (512, 512, 512, 8, 256, 256, 256, 64, 64, 128, 256, 256, 256, 256)
IN_OFF = np.concatenate([[0], np.cumsum(IN_SPL)])


def host_layer_inputs(inp, l, j):
    w_in = np.asarray(inp["w_in"][l])
    o = IN_OFF
    fq, fk, fv, ff, rr, rk, rv, rwd, rad, rgd, tq, tk, tv, tg = [w_in[:, o[i]:o[i + 1]] for i in range(14)]
    h0, h1 = 2 * j, 2 * j + 1
    sl = lambda w, h: w[:, 64 * h:64 * h + 64]
    sw = lambda w: np.concatenate([w[:, 32:64], w[:, 0:32]], 1)
    cols = [sl(fq, h0), sl(fq, h1), sl(fk, h0), sl(fk, h1),
            sl(tq, j), sw(sl(tq, j)), sl(tk, j), sw(sl(tk, j)),
            sl(fv, h0), sl(fv, h1), ff[:, h0:h0 + 2], sl(tv, j), sl(tg, j),
            sl(rr, j), sl(rk, j), rwd, rad, rgd, sl(rv, j)]
    W = np.ascontiguousarray(np.concatenate(cols, 1), dtype=np.float32)
    assert W.shape[1] == NCOL
    mr = np.asarray(inp["rwkv_mu_rkv"][l]); ml = np.asarray(inp["rwkv_mu_lora"][l])
    c64 = slice(64 * j, 64 * j + 64)
    mu = np.concatenate([mr[0, c64], mr[1, c64], ml, mr[2, c64]])[None, :].astype(np.float32)
    pcol = np.zeros((64, 8), np.float32)
    pcol[:, 0] = np.asarray(inp["rwkv_w0"][l])[c64]
    pcol[:, 1] = np.asarray(inp["rwkv_a0"][l])[c64]
    pcol[:, 2] = np.asarray(inp["rwkv_k_k"][l])[c64]
    pcol[:, 3] = np.asarray(inp["rwkv_k_a"][l])[c64]
    pcol[:, 5] = np.asarray(inp["rwkv_r_k"][l])[j]
    prow = np.concatenate([np.asarray(inp["fox_fgate_bias"][l])[h0:h0 + 2],
                           np.asarray(inp["rwkv_ln_g"][l])[c64], np.asarray(inp["rwkv_ln_b"][l])[c64],
                           np.asarray(inp["ret_gn_g"][l])[c64]])[None, :].astype(np.float32)
    return dict(W=W, mu=mu, pcol=pcol, prow=prow,
                w2h=np.ascontiguousarray(np.asarray(inp["rwkv_w2"][l])[:, c64]),
                a2h=np.ascontiguousarray(np.asarray(inp["rwkv_a2"][l])[:, c64]),
                g2h=np.ascontiguousarray(np.asarray(inp["rwkv_g2"][l])[:, c64]))

import numpy as np

ALPHA = (2.0 * 4) ** 0.25
LN_EPS = 1e-5
NE = 32


def build_B(NTOK, GN):
    NGR = NTOK // GN
    NTL = GN // 128
    HB = min(512, GN)
    NHB = GN // HB
    K = Kern()
    ycT = K.dram("ycT", [1024, NTOK], F32, "ExternalInput")
    xin = K.dram("xin", [NTOK, 1024], F32, "ExternalInput")
    wout = K.dram("wout", [1024, 1024], F32, "ExternalInput")
    lnp = K.dram("lnp", [1, 4096], F32, "ExternalInput")
    wr = K.dram("wr", [1024, 36], F32, "ExternalInput")
    br = K.dram("br", [1, 36], F32, "ExternalInput")
    w1 = K.dram("w1", [NE, 1024, 512], F32, "ExternalInput")
    w3 = K.dram("w3", [NE, 1024, 512], F32, "ExternalInput")
    w2 = K.dram("w2", [NE, 512, 1024], F32, "ExternalInput")
    idn = K.dram("idn", [128, 128], F32, "ExternalInput")
    out = K.dram("out", [NTOK, 1024], F32, "ExternalOutput")

    sb = K.sbuf
    ident = sb("ident", [128, 128], F32)
    lnb = sb("lnb", [128, 4096], F32)
    wrb = sb("wrb", [128, 8, 36], F32)
    brb = sb("brb", [128, 36], F32)
    woutb = sb("woutb", [128, 8, 1024], BF16)
    stg = [sb(f"stg{i}", [128, 2048], F32) for i in range(2)]
    w1b = [sb(f"w1b{i}", [128, 8, 512], BF16) for i in range(2)]
    w3b = [sb(f"w3b{i}", [128, 8, 512], BF16) for i in range(2)]
    w2b = [sb(f"w2b{i}", [128, 4, 1024], BF16) for i in range(2)]
    x1 = sb("x1", [128, NTL, 1024], F32)
    yacc = sb("yacc", [128, NTL, 1024], F32)
    x1Tb = sb("x1Tb", [128, 8, GN], BF16)
    x1Tf = sb("x1Tf", [128, 8, 128], F32)
    ycb = sb("ycb", [128, 8, 128], BF16)
    hb = sb("hb", [128, 1024], F32)
    hc = sb("hc", [128, 1024], F32)
    gates = sb("gates", [128, NTL, NE], F32)
    lg = sb("lg", [128, 36], F32)
    sm = [sb(f"sm{i}", [128, 8], F32) for i in range(10)]
    mg = sb("mg", [128, 4], F32)
    esel = sb("esel", [128, 8], F32); e2 = sb("e2", [128, 8], F32)
    tmp32 = sb("tmp32", [128, 32], F32)
    ge = sb("ge", [128, 8], F32)
    st = [sb(f"lnst{i}", [128, 1], F32) for i in range(4)]
    sil = [sb(f"sil{i}", [128, HB], BF16) for i in range(2)]
    hg = [sb(f"hg{i}", [128, NHB, HB], BF16) for i in range(4)]

    P = [K.psum(f"P{i}", [128, 512], F32) for i in range(8)]

    K.dma("sp", ident, idn)
    K.dma("sp", lnb, V(lnp.ap.partition_broadcast(128), lnp.bufs))
    K.dma("sp", brb, V(br.ap.partition_broadcast(128), br.bufs))
    K.dma("sp", wrb, vr(wr, "(kt p) c -> p kt c", p=128))
    wov = vr(wout, "(kt p) c -> p kt c", p=128)
    for q in range(4):
        sv = vr(stg[q % 2], "p (k c) -> p k c", k=2)
        K.dma("sp", sv, wov[:, 2 * q:2 * q + 2, :])
        K.copy(woutb[:, 2 * q:2 * q + 2, :], sv, eng=("act" if q % 2 == 0 else "dve"))

    def layernorm(dst, src, goff, boff):
        K.reduce(st[0], src, ALU.add)
        K.ts(st[0], st[0], -1.0 / 1024, ALU.mult)
        K.ts(hc, src, st[0], ALU.add)
        K.act(dst, hc, AF.Square, accum_out=st[1])
        K.ts(st[1], st[1], 1.0 / 1024, ALU.mult, LN_EPS, ALU.add)
        K.act(st[1], st[1], AF.Ln)
        K.act(st[1], st[1], AF.Exp, scale=-0.5)
        K.ts(hc, hc, st[1], ALU.mult)
        K.tt(hc, hc, lnb[:, goff:goff + 1024], ALU.mult)
        K.tt(dst, hc, lnb[:, boff:boff + 1024], ALU.add)

    ycTv = vr(ycT, "(kt p) s -> p kt s", p=128)
    xinv = vr(xin, "(n p) d -> n p d", p=128)
    outv = vr(out, "(n p) d -> n p d", p=128)
    w1v = vr(w1, "e (kt p) c -> e p kt c", p=128)
    w3v = vr(w3, "e (kt p) c -> e p kt c", p=128)
    w2v = vr(w2, "e (kt p) c -> e p kt c", p=128)
    out_toks = []
    wcount = [0]

    for gr in range(NGR):
        for tl in range(NTL):
            tile = gr * NTL + tl
            tok0 = tile * 128
            sv = vr(stg[0][:, 0:1024], "p (k c) -> p k c", k=8)
            K.dma("sp", sv, ycTv[:, :, tok0:tok0 + 128])
            K.copy(ycb, sv, eng="act")
            K.dma("sp", stg[1][:, 0:1024], xinv[tile])
            for half in range(2):
                for kt in range(8):
                    K.mm(P[half], ycb[:, kt, :], woutb[:, kt, 512 * half:512 * half + 512], start=(kt == 0), stop=(kt == 7))
            for half in range(2):
                K.stt(hb[:, 512 * half:512 * half + 512], stg[1][:, 512 * half:512 * half + 512], ALPHA, P[half], ALU.mult, ALU.add)
            layernorm(x1[:, tl, :], hb, 0, 1024)
            for half in range(2):
                for k4 in range(4):
                    kt = half * 4 + k4
                    K.transpose(P[2 + half][:, k4 * 128:(k4 + 1) * 128], x1[:, tl, kt * 128:(kt + 1) * 128], ident)
                K.copy(vr(x1Tf[:, 4 * half:4 * half + 4, :], "p k t -> p (k t)"), P[2 + half], eng=("act" if half == 0 else "dve"))
            K.copy(x1Tb[:, :, tl * 128:(tl + 1) * 128], x1Tf, eng="pool")
            for kt in range(8):
                K.mm(P[4][:, 0:36], x1Tf[:, kt, :], wrb[:, kt, :], start=(kt == 0), stop=(kt == 7))
            K.tt(lg, P[4][:, 0:36], brb, ALU.add)
            gl = lg[:, 0:4]
            gmax, gsum, m1, m2, wa, wb_, gg = sm[0][:, 0:1], sm[1][:, 0:1], sm[2][:, 0:1], sm[3][:, 0:1], sm[4][:, 0:1], sm[5][:, 0:1], sm[6][:, 0:1]
            K.reduce(gmax, gl, ALU.max)
            K.ts(mg, gl, gmax, ALU.is_equal)
            K.ts(sm[7][:, 0:4], gl, gmax, ALU.subtract)
            K.act(sm[7][:, 0:4], sm[7][:, 0:4], AF.Exp, accum_out=gsum)
            K.recip(gg, gsum)
            el = vr(lg[:, 4:36], "p (g e) -> p g e", g=4)
            K.tt(vr(tmp32, "p (g e) -> p g e", g=4), el, vb(vr(mg, "p (g o) -> p g o", o=1), [128, 4, 8]), ALU.mult)
            K.reduce(esel, vr(tmp32, "p (g e) -> p e g", g=4), ALU.add)
            K.reduce(m1, esel, ALU.max)
            K.ts(sm[8], esel, m1, ALU.is_equal)
            K.stt(e2, sm[8], -1e30, esel, ALU.mult, ALU.add)
            K.reduce(m2, e2, ALU.max)
            K.ts(sm[9], e2, m2, ALU.is_equal)
            K.tt(wa, m2, m1, ALU.subtract)
            K.act(wa, wa, AF.Exp)
            K.ts(wa, wa, 1.0, ALU.add)
            K.recip(wa, wa)
            K.ts(wb_, wa, -1.0, ALU.mult, 1.0, ALU.add)
            K.tt(wa, wa, gg, ALU.mult)
            K.tt(wb_, wb_, gg, ALU.mult)
            K.ts(ge, sm[8], wa, ALU.mult)
            K.stt(ge, sm[9], wb_, ge, ALU.mult, ALU.add)
            K.tt(vr(gates[:, tl, :], "p (g e) -> p g e", g=4), vb(vr(mg, "p (g o) -> p g o", o=1), [128, 4, 8]),
                 vb(vr(ge, "p (o e) -> p o e", o=1), [128, 4, 8]), ALU.mult)
        for e in range(NE):
            wi = wcount[0] % 2; wcount[0] += 1
            for (src, dstb, k4n, eng) in ((w1v, w1b, 8, "act"), (w3v, w3b, 8, "dve")):
                for hq in range(2):
                    s_ = stg[hq]
                    sv = vr(s_[:, 0:2048], "p (k c) -> p k c", k=4)
                    K.dma("sp", sv, src[e][:, 4 * hq:4 * hq + 4, :])
                    K.copy(dstb[wi][:, 4 * hq:4 * hq + 4, :], sv, eng=(eng if hq == 0 else "pool"))
            for hq in range(2):
                sv = vr(stg[hq][:, 0:2048], "p (k c) -> p k c", k=2)
                K.dma("sp", sv, w2v[e][:, 2 * hq:2 * hq + 2, :])
                K.copy(w2b[wi][:, 2 * hq:2 * hq + 2, :], sv, eng=("act" if hq == 0 else "dve"))
            for hbk in range(NHB):
                cs_ = slice(hbk * HB, (hbk + 1) * HB)
                for nt in range(4):
                    pa, pb = P[nt % 2], P[2 + nt % 2]
                    for kt in range(8):
                        K.mm(pa[:, 0:HB], w1b[wi][:, kt, nt * 128:(nt + 1) * 128], x1Tb[:, kt, cs_], start=(kt == 0), stop=(kt == 7))
                    for kt in range(8):
                        K.mm(pb[:, 0:HB], w3b[wi][:, kt, nt * 128:(nt + 1) * 128], x1Tb[:, kt, cs_], start=(kt == 0), stop=(kt == 7))
                    s2 = sil[nt % 2]
                    K.act(s2, pa[:, 0:HB], AF.Silu)
                    K.tt(hg[nt][:, hbk, :], s2, pb[:, 0:HB], ALU.mult)
            for tl in range(NTL):
                hbk, off = (tl * 128) // HB, (tl * 128) % HB
                for half in range(2):
                    py = P[4 + 2 * (tl % 2) + half]
                    for kt in range(4):
                        K.mm(py, hg[kt][:, hbk, off:off + 128], w2b[wi][:, kt, 512 * half:512 * half + 512], start=(kt == 0), stop=(kt == 3))
                    ysl = yacc[:, tl, 512 * half:512 * half + 512]
                    if e == 0:
                        K.ts(ysl, py, gates[:, tl, e:e + 1], ALU.mult)
                    else:
                        K.stt(ysl, py, gates[:, tl, e:e + 1], ysl, ALU.mult, ALU.add)
        for tl in range(NTL):
            tile = gr * NTL + tl
            K.stt(hb, x1[:, tl, :], ALPHA, yacc[:, tl, :], ALU.mult, ALU.add)
            layernorm(yacc[:, tl, :], hb, 2048, 3072)
            yo = V(outv.ap[tile], [Buf()])
            out_toks.append(K.dma("pool", yo, yacc[:, tl, :], owner=yacc.bufs[0]))
    K.finish(out_toks)
    return K


_CACHE = {}


def _get_A(S):
    if ("A", S) not in _CACHE:
        _CACHE[("A", S)] = build_A(S)
    return _CACHE[("A", S)]


def _get_B(NTOK, GN):
    if ("B", NTOK, GN) not in _CACHE:
        _CACHE[("B", NTOK, GN)] = build_B(NTOK, GN)
    return _CACHE[("B", NTOK, GN)]


def host_B(inp, l):
    return dict(wout=np.ascontiguousarray(inp["w_out"][l]),
                lnp=np.concatenate([inp["ln1_g"][l], inp["ln1_b"][l], inp["ln2_g"][l], inp["ln2_b"][l]])[None, :].astype(np.float32),
                wr=np.ascontiguousarray(np.concatenate([inp["moe_w_group"][l], inp["moe_w_expert"][l]], 1)),
                br=np.concatenate([inp["moe_b_group"][l], inp["moe_b_expert"][l]])[None, :].astype(np.float32),
                w1=np.ascontiguousarray(inp["moe_w1"][l]), w3=np.ascontiguousarray(inp["moe_w3"][l]),
                w2=np.ascontiguousarray(inp["moe_w2"][l]),
                idn=np.eye(128, dtype=np.float32))


def kernel(**inputs):
    inp = {k: np.asarray(v) for k, v in inputs.items()}
    x = np.ascontiguousarray(inp["x"], dtype=np.float32)
    B_, S_, D = x.shape
    NC = 8
    T = B_ * S_
    NTOK = T // NC
    depth = inp["w_in"].shape[0]
    consts = [host_consts(S_, j) for j in range(4)]
    for l in range(depth):
        KA = _get_A(S_)
        xTs = [np.ascontiguousarray(x[b].T) for b in range(B_)]
        in_maps = []
        for c in range(NC):
            b, j = c // 4, c % 4
            d = dict(xT=xTs[b])
            d.update(consts[j])
            d.update(host_layer_inputs(inp, l, j))
            in_maps.append(d)
        res = run_bass_kernel_spmd(KA.nc, in_maps, core_ids=list(range(NC)), trace=True)
        ycat = np.empty((B_, S_, D), np.float32)
        for c in range(NC):
            b, j = c // 4, c % 4
            yk = res.results[c]["y"]
            ycat[b, :, 128 * j:128 * j + 128] = yk[:, 0:128]
            ycat[b, :, 512 + 64 * j:512 + 64 * j + 64] = yk[:, 128:192]
            ycat[b, :, 768 + 64 * j:768 + 64 * j + 64] = yk[:, 192:256]
        KB = _get_B(NTOK, 1024)
        hb = host_B(inp, l)
        yf = ycat.reshape(T, D)
        xf = x.reshape(T, D)
        in_maps = []
        for c in range(NC):
            sl = slice(c * NTOK, (c + 1) * NTOK)
            d = dict(ycT=np.ascontiguousarray(yf[sl].T), xin=np.ascontiguousarray(xf[sl]))
            d.update(hb)
            in_maps.append(d)
        res = run_bass_kernel_spmd(KB.nc, in_maps, core_ids=list(range(NC)), trace=True)
        x = np.concatenate([res.results[c]["out"] for c in range(NC)], 0).reshape(B_, S_, D)
    return x.astype(np.float32)
```
